# Optimizing a Trainium2 kernel written in Bass

```python
import jax, jax.numpy as jnp
from jax import lax
import numpy as np

D_MODEL = 2048
BATCH = 2
SEQ = 4096
DEPTH = 1

D_MIX = D_MODEL
D_ATT = D_MIX // 2
ATT_HEAD_DIM = 128
ATT_HEADS = D_ATT // ATT_HEAD_DIM
ROPE_THETA = 10000.0
MOBA_BLOCK = 256
MOBA_TOPK = 3
MOBA_Q_CHUNK = 32
D_RWKV = D_MIX - D_ATT
RWKV_HEAD_DIM = 64
RWKV_HEADS = D_RWKV // RWKV_HEAD_DIM
D_DECAY_LORA = 64
D_AAA_LORA = 64
D_GATE_LORA = 160
D_RWKV_IN = 3 * D_RWKV + D_DECAY_LORA + D_AAA_LORA + D_GATE_LORA
D_IN = 3 * D_ATT + D_RWKV_IN
N_EXPERTS = 32
TOP_K = 4
D_EXPERT = D_MODEL
SWIGLU_LIMIT = 7.0
SWIGLU_ALPHA = 1.702
MOE_BLOCK = 128
LN_EPS = 1e-5
GN_EPS = 64e-5
NEG_INF = -1e30
DEEPNORM_ALPHA = (2 * DEPTH) ** 0.25
DEEPNORM_BETA = (8 * DEPTH) ** -0.25

kernel_name = 'hybrid_moba_rwkv7_moe_deepnorm'


def _layer_norm(x, g, b):
    xf = x.astype(jnp.float32)
    mu = xf.mean(-1, keepdims=True)
    var = jnp.square(xf - mu).mean(-1, keepdims=True)
    return ((xf - mu) * lax.rsqrt(var + LN_EPS) * g + b).astype(x.dtype)


def _rope(t):
    S, Dh = t.shape[2], t.shape[3]
    half = Dh // 2
    inv = ROPE_THETA ** (-jnp.arange(0, Dh, 2, dtype=jnp.float32) / Dh)
    ang = jnp.arange(S, dtype=jnp.float32)[:, None] * inv[None, :]
    cos = jnp.concatenate([jnp.cos(ang), jnp.cos(ang)], axis=-1)
    sin = jnp.concatenate([jnp.sin(ang), jnp.sin(ang)], axis=-1)
    tf = t.astype(jnp.float32)
    rot = jnp.concatenate([-tf[..., half:], tf[..., :half]], axis=-1)
    return (tf * cos + rot * sin).astype(t.dtype)


def _moba_attention(q, k, v):
    B, H, S, Dh = q.shape
    nb = -(-S // MOBA_BLOCK)
    pad = nb * MOBA_BLOCK - S
    kp = jnp.pad(k, ((0, 0), (0, 0), (0, pad), (0, 0)))
    vp = jnp.pad(v, ((0, 0), (0, 0), (0, pad), (0, 0)))
    kb = kp.reshape(B, H, nb, MOBA_BLOCK, Dh)
    vb = vp.reshape(B, H, nb, MOBA_BLOCK, Dh)
    k_mean = kb.astype(jnp.float32).mean(axis=3)
    n_sel = min(MOBA_TOPK, nb)
    n_chunks = S // MOBA_Q_CHUNK
    q_chunks = jnp.moveaxis(q.reshape(B, H, n_chunks, MOBA_Q_CHUNK, Dh), 2, 0)
    b_idx = jnp.arange(B)[:, None, None, None]
    h_idx = jnp.arange(H)[None, :, None, None]
    blk_ids = jnp.arange(nb)
    scale = Dh ** -0.5

    def one_chunk(args):
        q_blk, ci = args
        q0 = ci * MOBA_Q_CHUNK
        cur = q0 // MOBA_BLOCK
        q_pos = q0 + jnp.arange(MOBA_Q_CHUNK)
        gate = jnp.einsum('bhqd,bhnd->bhqn', q_blk.astype(jnp.float32), k_mean)
        gate = jnp.where(blk_ids < cur, gate, NEG_INF)
        _, sel = lax.top_k(gate, n_sel)
        sel_ok = sel < cur
        k_sel = kb[b_idx, h_idx, sel]
        v_sel = vb[b_idx, h_idx, sel]
        s_sel = jnp.einsum('bhqd,bhqnkd->bhqnk', q_blk, k_sel).astype(jnp.float32) * scale
        s_sel = jnp.where(sel_ok[..., None], s_sel, NEG_INF)
        k_own = lax.dynamic_slice_in_dim(kp, cur * MOBA_BLOCK, MOBA_BLOCK, axis=2)
        v_own = lax.dynamic_slice_in_dim(vp, cur * MOBA_BLOCK, MOBA_BLOCK, axis=2)
        s_own = jnp.einsum('bhqd,bhkd->bhqk', q_blk, k_own).astype(jnp.float32) * scale
        k_pos = cur * MOBA_BLOCK + jnp.arange(MOBA_BLOCK)
        s_own = jnp.where(k_pos[None, :] <= q_pos[:, None], s_own, NEG_INF)
        s_all = jnp.concatenate([s_sel.reshape(B, H, MOBA_Q_CHUNK, n_sel * MOBA_BLOCK), s_own], axis=-1)
        p = jax.nn.softmax(s_all, axis=-1).astype(v.dtype)
        p_sel = p[..., :n_sel * MOBA_BLOCK].reshape(B, H, MOBA_Q_CHUNK, n_sel, MOBA_BLOCK)
        p_own = p[..., n_sel * MOBA_BLOCK:]
        return (jnp.einsum('bhqnk,bhqnkd->bhqd', p_sel, v_sel)
                + jnp.einsum('bhqk,bhkd->bhqd', p_own, v_own))

    out = lax.map(one_chunk, (q_chunks, jnp.arange(n_chunks)))
    return out.transpose(1, 0, 3, 2, 4).reshape(B, S, H * Dh)


def _rwkv7_scan(r, w, k, v, a, b):
    B, T, H, N = r.shape

    def step(state, inp):
        r_t, w_t, k_t, v_t, a_t, b_t = inp
        sa = jnp.einsum('bhij,bhj->bhi', state, a_t)
        state = (state * w_t[:, :, None, :] + sa[..., None] * b_t[:, :, None, :]
                 + v_t[..., None] * k_t[:, :, None, :])
        return state, jnp.einsum('bhij,bhj->bhi', state, r_t)

    xs = tuple(jnp.moveaxis(t, 1, 0) for t in (r, w, k, v, a, b))
    _, ys = lax.scan(step, jnp.zeros((B, H, N, N), jnp.float32), xs)
    return jnp.moveaxis(ys, 0, 1)


def _rwkv7_mixer(p_r, p_k, p_v, p_w, p_a, p_g, w0, w_up, a0, a_up, g_up, k_k, k_a, r_k, lnx_g, lnx_b):
    B, T, _ = p_r.shape
    H, N = RWKV_HEADS, RWKV_HEAD_DIM
    f32 = jnp.float32
    w = -jax.nn.softplus(-(w0 + jnp.tanh(p_w) @ w_up).astype(f32)) - 0.5
    decay = jnp.exp(-jnp.exp(w))
    a = jax.nn.sigmoid((a0 + p_a @ a_up).astype(f32))
    g = jax.nn.sigmoid(p_g) @ g_up
    kk = (p_k * k_k).astype(f32).reshape(B, T, H, N)
    kk = kk / jnp.maximum(jnp.sqrt(jnp.sum(kk * kk, axis=-1, keepdims=True)), 1e-12)
    k = p_k.astype(f32) * (1.0 + (a - 1.0) * k_a)
    r4 = p_r.astype(f32).reshape(B, T, H, N)
    k4 = k.reshape(B, T, H, N)
    v4 = p_v.astype(f32).reshape(B, T, H, N)
    a4 = a.reshape(B, T, H, N)
    y = _rwkv7_scan(r4, decay.reshape(B, T, H, N), k4, v4, -kk, kk * a4)
    mu = y.mean(-1, keepdims=True)
    var = jnp.square(y - mu).mean(-1, keepdims=True)
    y = ((y - mu) * lax.rsqrt(var + GN_EPS)).reshape(B, T, D_RWKV) * lnx_g + lnx_b
    bonus = jnp.sum(r4 * k4 * r_k, axis=-1, keepdims=True) * v4
    y = y + bonus.reshape(B, T, D_RWKV)
    return (y * g).astype(p_r.dtype)


def _moe(h, layer, router_w, router_b, w_gu, b_gu, w_down, b_down):
    N, D = h.shape
    logits = (h @ router_w[layer] + router_b[layer]).astype(jnp.float32)
    top_logit, top_idx = lax.top_k(logits, TOP_K)
    gates = jax.nn.softmax(top_logit, axis=-1)
    A = N * TOP_K
    flat_e = top_idx.reshape(A)
    flat_tok = jnp.repeat(jnp.arange(N, dtype=jnp.int32), TOP_K)
    flat_gate = gates.reshape(A)
    order = jnp.argsort(flat_e)
    se, st, sg = flat_e[order], flat_tok[order], flat_gate[order]
    counts = jnp.bincount(flat_e, length=N_EXPERTS)
    padded = (counts + MOE_BLOCK - 1) // MOE_BLOCK * MOE_BLOCK
    starts = jnp.cumsum(counts) - counts
    p_ends = jnp.cumsum(padded)
    p_starts = p_ends - padded
    dest = p_starts[se] + (jnp.arange(A) - starts[se])
    n_blocks = (A + N_EXPERTS * (MOE_BLOCK - 1) + MOE_BLOCK - 1) // MOE_BLOCK
    rows = n_blocks * MOE_BLOCK
    row_tok = jnp.zeros((rows,), jnp.int32).at[dest].set(st)
    block_e = jnp.minimum(jnp.searchsorted(p_ends, jnp.arange(n_blocks) * MOE_BLOCK, side='right'),
                          N_EXPERTS - 1)

    def run_block(args):
        tok, e = args
        xb = h[tok]
        gu = xb @ w_gu[layer, e] + b_gu[layer, e]
        gate = jnp.minimum(gu[:, :D_EXPERT], SWIGLU_LIMIT)
        up = jnp.clip(gu[:, D_EXPERT:], -SWIGLU_LIMIT, SWIGLU_LIMIT)
        glu = gate * jax.nn.sigmoid(gate * SWIGLU_ALPHA)
        return ((up + 1.0) * glu) @ w_down[layer, e] + b_down[layer, e]

    ys = lax.map(run_block, (row_tok.reshape(n_blocks, MOE_BLOCK), block_e)).reshape(rows, D)
    contrib = ys[dest] * sg[:, None].astype(ys.dtype)
    return jnp.zeros((N, D), ys.dtype).at[st].add(contrib)


def setup_inputs(seed: int = 0) -> dict:
    key = jax.random.key(seed)
    ks = jax.random.split(key, 24)
    L = DEPTH

    def nrm(k, shape, s):
        return jax.random.normal(k, shape, jnp.float32) * s

    x = nrm(ks[0], (BATCH, SEQ, D_MODEL), 1.0)
    col_scale = jnp.concatenate([
        jnp.ones((2 * D_ATT,), jnp.float32), jnp.full((D_ATT,), DEEPNORM_BETA, jnp.float32),
        jnp.ones((2 * D_RWKV,), jnp.float32), jnp.full((D_RWKV,), DEEPNORM_BETA, jnp.float32),
        jnp.ones((D_DECAY_LORA + D_AAA_LORA + D_GATE_LORA,), jnp.float32)])
    w_in = nrm(ks[1], (L, D_MODEL, D_IN), D_MODEL ** -0.5) * col_scale
    mu_shift = jax.random.uniform(ks[2], (L, D_RWKV_IN), jnp.float32)
    w0 = -6.0 + 5.0 * jax.random.uniform(ks[3], (L, D_RWKV), jnp.float32)
    w_up = nrm(ks[4], (L, D_DECAY_LORA, D_RWKV), 0.5 * D_DECAY_LORA ** -0.5)
    a0 = nrm(ks[5], (L, D_RWKV), 0.1)
    a_up = nrm(ks[6], (L, D_AAA_LORA, D_RWKV), D_AAA_LORA ** -0.5)
    g_up = nrm(ks[7], (L, D_GATE_LORA, D_RWKV), D_GATE_LORA ** -0.5)
    k_k = 0.85 + nrm(ks[8], (L, D_RWKV), 0.05)
    k_a = 1.0 + nrm(ks[9], (L, D_RWKV), 0.05)
    r_k = nrm(ks[10], (L, RWKV_HEADS, RWKV_HEAD_DIM), 0.1)
    lnx_g = 1.0 + nrm(ks[11], (L, D_RWKV), 0.05)
    lnx_b = nrm(ks[12], (L, D_RWKV), 0.02)
    w_out = nrm(ks[13], (L, D_MIX, D_MODEL), DEEPNORM_BETA * D_MIX ** -0.5)
    ln1_g = 1.0 + nrm(ks[14], (L, D_MODEL), 0.05)
    ln1_b = nrm(ks[15], (L, D_MODEL), 0.02)
    router_w = nrm(ks[16], (L, D_MODEL, N_EXPERTS), D_MODEL ** -0.5)
    router_b = nrm(ks[17], (L, N_EXPERTS), 0.01)
    w_gu = nrm(ks[18], (L, N_EXPERTS, D_MODEL, 2 * D_EXPERT), D_MODEL ** -0.5)
    b_gu = nrm(ks[19], (L, N_EXPERTS, 2 * D_EXPERT), 0.01)
    w_down = nrm(ks[20], (L, N_EXPERTS, D_EXPERT, D_MODEL), DEEPNORM_BETA * D_EXPERT ** -0.5)
    b_down = nrm(ks[21], (L, N_EXPERTS, D_MODEL), 0.01)
    ln2_g = 1.0 + nrm(ks[22], (L, D_MODEL), 0.05)
    ln2_b = nrm(ks[23], (L, D_MODEL), 0.02)
    return {'x': x, 'w_in': w_in, 'mu_shift': mu_shift, 'w0': w0, 'w_up': w_up, 'a0': a0,
            'a_up': a_up, 'g_up': g_up, 'k_k': k_k, 'k_a': k_a, 'r_k': r_k, 'lnx_g': lnx_g,
            'lnx_b': lnx_b, 'w_out': w_out, 'ln1_g': ln1_g, 'ln1_b': ln1_b, 'router_w': router_w,
            'router_b': router_b, 'w_gu': w_gu, 'b_gu': b_gu, 'w_down': w_down, 'b_down': b_down,
            'ln2_g': ln2_g, 'ln2_b': ln2_b}


def reference(x, w_in, mu_shift, w0, w_up, a0, a_up, g_up, k_k, k_a, r_k, lnx_g, lnx_b, w_out,
              ln1_g, ln1_b, router_w, router_b, w_gu, b_gu, w_down, b_down, ln2_g, ln2_b):
    B, S, D = x.shape
    c0 = 3 * D_RWKV
    for l in range(DEPTH):
        proj = x @ w_in[l]
        q = proj[..., :D_ATT].reshape(B, S, ATT_HEADS, ATT_HEAD_DIM).transpose(0, 2, 1, 3)
        k = proj[..., D_ATT:2 * D_ATT].reshape(B, S, ATT_HEADS, ATT_HEAD_DIM).transpose(0, 2, 1, 3)
        v = proj[..., 2 * D_ATT:3 * D_ATT].reshape(B, S, ATT_HEADS, ATT_HEAD_DIM).transpose(0, 2, 1, 3)
        attn = _moba_attention(_rope(q), _rope(k), v)
        rw = proj[..., 3 * D_ATT:]
        rw_prev = jnp.pad(rw[:, :-1], ((0, 0), (1, 0), (0, 0)))
        rw = rw + (rw_prev - rw) * mu_shift[l]
        p_r = rw[..., :D_RWKV]
        p_k = rw[..., D_RWKV:2 * D_RWKV]
        p_v = rw[..., 2 * D_RWKV:c0]
        p_w = rw[..., c0:c0 + D_DECAY_LORA]
        p_a = rw[..., c0 + D_DECAY_LORA:c0 + D_DECAY_LORA + D_AAA_LORA]
        p_g = rw[..., c0 + D_DECAY_LORA + D_AAA_LORA:]
        rwkv = _rwkv7_mixer(p_r, p_k, p_v, p_w, p_a, p_g, w0[l], w_up[l], a0[l], a_up[l], g_up[l],
                            k_k[l], k_a[l], r_k[l], lnx_g[l], lnx_b[l])
        mix = jnp.concatenate([attn, rwkv], axis=-1) @ w_out[l]
        x = _layer_norm(DEEPNORM_ALPHA * x + mix, ln1_g[l], ln1_b[l])
        ffn = _moe(x.reshape(B * S, D), l, router_w, router_b, w_gu, b_gu, w_down, b_down).reshape(B, S, D)
        x = _layer_norm(DEEPNORM_ALPHA * x + ffn, ln2_g[l], ln2_b[l])
    return x
```

```python
import os
import numpy as np
import ml_dtypes
from contextlib import ExitStack
import concourse.bass as bass
import concourse.mybir as mybir
from concourse.bass_utils import run_bass_kernel_spmd

F32 = mybir.dt.float32
BF16 = mybir.dt.bfloat16
AF = mybir.ActivationFunctionType
OP = mybir.AluOpType
AX = mybir.AxisListType

T = 4096
TS = 256
NSUB = 2
NTILE = 16
D = 2048
NEG = -1.0e30
ALPHA = 2.0 ** 0.25
LD_SCALE = float(np.exp(-0.5))
ENG = ['pe', 'act', 'dve', 'pool', 'sp']
BLK = {'pe': 'tensor', 'act': 'scalar', 'dve': 'vector', 'pool': 'gpsimd', 'sp': 'sync'}


class Prog:
    def __init__(s, nc, es):
        s.nc = nc
        s.es = es
        s.ops = []
        s.lastw = {}
        s.readers = {}
        s.esem = {e: es.enter_context(nc.semaphore('s_' + e)) for e in ENG}
        s.ecnt = {e: 0 for e in ENG}
        s.dsem = {}
        s.dcnt = {}
        s.flushed = 0
        s.waited = {e: {} for e in ENG}
        s.fence = []
        s.rr = 0
        s.dhist = {}

    def op(s, eng, fn, r=(), w=(), dma=None):
        idx = len(s.ops)
        if idx >= int(os.environ.get('MAXOPS', 10 ** 9)):
            return -1
        deps = set()
        for x in r:
            if x in s.lastw:
                deps.add(s.lastw[x])
            if x.startswith('ps'):
                for rd in s.readers.get(x, ()):
                    if s.ops[rd]['eng'] != eng:
                        deps.add(rd)
        for x in w:
            rds = s.readers.get(x, ())
            if x in s.lastw:
                lw = s.ops[s.lastw[x]]
                if not (dma is not None and lw['dma'] == dma and len(rds) > 0):
                    deps.add(s.lastw[x])
            deps.update(rds)
        for x in r:
            s.readers.setdefault(x, []).append(idx)
        for x in w:
            s.lastw[x] = idx
            s.readers[x] = []
        if dma is not None and dma not in s.dsem:
            s.dsem[dma] = s.es.enter_context(s.nc.semaphore('d_' + dma))
            s.dcnt[dma] = 0
        s.ops.append(dict(eng=eng, fn=fn, deps=deps, dma=dma, needed=False, idx=idx))
        return idx

    def flush(s):
        nc = s.nc
        lo = s.flushed
        ops = s.ops[lo:]
        for o in ops:
            best = {}
            for d in o['deps']:
                if d < lo:
                    continue
                Dd = s.ops[d]
                if Dd['dma'] is not None:
                    continue
                if Dd['eng'] == 'pe' and o['eng'] == 'pe' and o['dma'] is None:
                    continue
                if d > best.get(Dd['eng'], -1):
                    best[Dd['eng']] = d
            for d in best.values():
                s.ops[d]['needed'] = True
        last = {}
        for i, o in enumerate(ops):
            if o['dma'] is None:
                last[o['eng']] = i
        for e, i in last.items():
            ops[i]['needed'] = True
        for gi, o in enumerate(ops, lo):
            if o['dma'] is not None:
                k = o['dma']
                s.dcnt[k] += 16
                o['sv'] = (s.dsem[k], s.dcnt[k], 'd' + k)
                s.dhist.setdefault(k, []).append((gi, s.dcnt[k]))
            elif o['needed']:
                s.ecnt[o['eng']] += 1
                o['sv'] = (s.esem[o['eng']], s.ecnt[o['eng']], 'e' + o['eng'])
        with nc.Block() as blk:
            for e in ENG:
                myops = [o for o in ops if o['eng'] == e]

                def body(eng, myops=myops, e=e):
                    waited = s.waited[e]
                    for (semh, val, key) in s.fence:
                        if val > 0 and waited.get(key, 0) < val:
                            eng.wait_ge(semh, val)
                            waited[key] = val
                    for o in myops:
                        oi = o['idx']
                        for d in sorted(o['deps']):
                            if d < lo:
                                continue
                            Dd = s.ops[d]
                            if 'sv' not in Dd:
                                continue
                            if Dd['dma'] is None and Dd['eng'] == 'pe' and e == 'pe' and o['dma'] is None:
                                continue
                            semh, val, key = Dd['sv']
                            if Dd['dma'] is not None:
                                for (gi2, v2) in s.dhist[Dd['dma']]:
                                    if gi2 < oi and v2 > val:
                                        val = v2
                            if waited.get(key, 0) < val:
                                eng.wait_ge(semh, val)
                                waited[key] = val
                        ins = o['fn'](eng)
                        if 'sv' in o:
                            ins.then_inc(o['sv'][0], 16 if o['dma'] is not None else 1)
                getattr(blk, BLK[e])(body)
        s.fence = [(s.esem[e], s.ecnt[e], 'e' + e) for e in ENG] + \
                  [(h, s.dcnt[k], 'd' + k) for k, h in s.dsem.items()]
        s.flushed = len(s.ops)

    def final_wait(s):
        with s.nc.Block() as blk:
            def body(eng):
                for (semh, val, key) in s.fence:
                    if val > 0:
                        eng.wait_ge(semh, val)
            blk.sync(body)


def build(dbg=None):
    nc = bass.Bass("TRN2", target_bir_lowering=False)

    def din(name, shape, dt=F32):
        return nc.dram_tensor(name, list(shape), dt, kind="ExternalInput").ap()

    x = din('x', [T, D])
    if dbg != 'p1':
        xres = din('xres', [1024, D])
    w_in = din('w_in_c', [D, 1824])
    mu_rv = din('mu_rv', [1, 256])
    pvec = din('pvec', [128, 20])
    rowv = din('rowv', [1, 512])
    w_up = din('w_up_c', [64, 256])
    a_up = din('a_up_c', [64, 256])
    g_up = din('g_up_c', [160, 256])
    if dbg != 'p1':
        w_out = din('w_out_p', [D, D])
        lnv = din('lnv', [1, 4 * D])
        router_w = din('router_w', [D, 32])
        router_b = din('router_b', [1, 32])
    if dbg is None:
        w_gu = din('w_gu', [32, D, 4096])
        bgu_p = din('bgu_p', [128, 32 * 32])
        w_down = din('w_down', [32, D, D])
        b_down = din('b_down', [32, D])
    cosT = din('cosT', [128, T])
    sinT = din('sinT', [128, T])
    cf32 = din('cf32', [128, 128 + 3 * 512 + 128 + 3 * 256])
    cbf = din('cbf', [128, 128 + 128 + 128 + 512 + 16 * 128], BF16)
    tokoff = None
    if dbg == 'p1':
        _dbgt = nc.dram_tensor('dbg', [512, T], BF16, kind="ExternalOutput").ap()
        agin = [_dbgt[f * 128:(f + 1) * 128, :] for f in range(4)]
    else:
        out = nc.dram_tensor('dbg2' if dbg == 'p2a' else 'out', [1024, D], F32, kind="ExternalOutput").ap()
        agin = [nc.dram_tensor('agin%d' % f, [128, T], BF16, kind="Internal").ap() for f in range(4)]
        agout = [nc.dram_tensor('agout%d' % f, [4 * 128, T], BF16, kind="Internal").ap() for f in range(4)]

    ges = ExitStack()
    P = Prog(nc, ges)
    ps = [ges.enter_context(nc.psum_tensor('ps%d' % i, [128, 512], F32)) for i in range(8)]
    rot_banks = [0, 1, 2, 3, 6, 7]

    def nb():
        P.rr = (P.rr + 1) % len(rot_banks)
        return rot_banks[P.rr]

    def sb(es, name, shape, dt=F32):
        return es.enter_context(nc.sbuf_tensor(name, list(shape), dt))

    def mm(o, l, r_, st, sp_, rd, wr):
        P.op('pe', lambda e: e.matmul(o, lhsT=l, rhs=r_, start=st, stop=sp_), rd, wr)

    def tr(o, i, idn, rd, wr):
        P.op('pe', lambda e: e.transpose(o, i, idn), rd, wr)

    def act(o, i, f, rd, wr, bias=None, scale=None, accum=None):
        kw = {}
        if bias is not None:
            kw['bias'] = bias
        if scale is not None:
            kw['scale'] = scale
        if accum is not None:
            kw['accum_out'] = accum
        P.op('act', lambda e: e.activation(out=o, in_=i, func=f, **kw), rd, wr)

    def tt(eng, o, a, b, op, rd, wr):
        P.op(eng, lambda e: e.tensor_tensor(out=o, in0=a, in1=b, op=op), rd, wr)

    def ts(eng, o, a, s1, s2, op0, op1, rd, wr):
        if s2 is None:
            P.op(eng, lambda e: e.tensor_scalar(out=o, in0=a, scalar1=s1, scalar2=None, op0=op0), rd, wr)
        else:
            P.op(eng, lambda e: e.tensor_scalar(out=o, in0=a, scalar1=s1, scalar2=s2, op0=op0, op1=op1), rd, wr)

    def stt(eng, o, a, sc, b, op0, op1, rd, wr):
        P.op(eng, lambda e: e.scalar_tensor_tensor(out=o, in0=a, scalar=sc, in1=b, op0=op0, op1=op1), rd, wr)

    def cp(eng, o, i, rd, wr):
        if eng == 'act':
            P.op('act', lambda e: e.copy(out=o, in_=i), rd, wr)
        else:
            P.op(eng, lambda e: e.tensor_copy(out=o, in_=i), rd, wr)

    def dma(o, i, rd, wr, key, q='sp'):
        P.op(q, lambda e: e.dma_start(out=o, in_=i), rd, wr, dma=key)

    def mset(eng, o, v, wr):
        P.op(eng, lambda e: e.memset(o, v), (), wr)

    CF = sb(ges, 'CF', [128, 128 + 3 * 512 + 128 + 3 * 256])
    CB = sb(ges, 'CB', [128, 128 + 128 + 128 + 512 + 16 * 128], BF16)
    dma(CF[:], cf32, (), ['CF'], 'c0')
    dma(CB[:], cbf, (), ['CB'], 'c1')
    identF = CF[:, 0:128]
    SU4 = CF[:, 128:640]
    SL4 = CF[:, 640:1152]
    UI4 = CF[:, 1152:1664]
    blockones = CF[:, 1664:1792]
    validb = CF[:, 1792:2048]
    ownb = CF[:, 2048:2304]
    futb = CF[:, 2304:2560]
    identB = CB[:, 0:128]
    rotB = CB[:, 128:256]
    onesB = CB[:, 256:384]
    causB = CB[:, 384:896]
    onehotB = CB[:, 896:896 + 2048]

    es = ExitStack()
    Wb = sb(es, 'Wb', [128, 16, 2080], BF16)
    Kt = sb(es, 'Kt', [128, 2, T], BF16)
    Vtok = sb(es, 'Vtok', [128, 32, 256], BF16)
    kms = sb(es, 'kms', [128, 2, 16])
    kmsB = sb(es, 'kmsB', [128, 2, 16], BF16)
    pv = sb(es, 'pv', [128, 24])
    rowb = sb(es, 'rowb', [128, 512])
    murv = sb(es, 'murv', [128, 512])
    wupS = sb(es, 'wupS', [128, 256])
    aupS = sb(es, 'aupS', [128, 256])
    gupS = sb(es, 'gupS', [128, 512])
    xs0 = sb(es, 'xs0', [128, D])
    xs = [xs0, xs0]
    xsn = ['xs0', 'xs0']
    xT = sb(es, 'xT', [128, 16, TS + 1], BF16)
    Mst = [sb(es, 'M%d' % i, [128, 2, 64]) for i in range(2)]
    _bk0 = sb(es, 'BK0', [128, 2, 2, 2, 128])
    BK = [_bk0, _bk0]

    dma(pv[:, 0:20], pvec, (), ['pv'], 'c2')
    dma(rowb[:], rowv.partition_broadcast(128), (), ['rowb'], 'c3')
    dma(murv[:, 0:256], mu_rv.partition_broadcast(128), (), ['murv'], 'c4')
    dma(wupS[0:64, :], w_up, (), ['wupS'], 'c5')
    dma(aupS[64:128, :], a_up, (), ['aupS'], 'c6')
    dma(gupS[:, 0:256], g_up[0:128, :], (), ['gupS'], 'c7')
    dma(gupS[0:32, 256:512], g_up[128:160, :], (), ['gupS2'], 'c7')
    pv2 = sb(es, 'pv2', [128, 16])
    ts('dve', pv[:, 17:24], pv[:, 0:7], -1.0, 1.0, OP.mult, OP.add, ['pv'], ['pvb'])
    ts('dve', pv2[:, 0:2], pv[:, 13:15], -1.0, 1.0, OP.mult, OP.add, ['pv'], ['pv2'])
    ts('dve', murv[:, 256:512], murv[:, 0:256], -1.0, 1.0, OP.mult, OP.add, ['murv'], ['murv2'])
    mset('dve', kms[:], 0.0, ['kms'])
    mset('dve', Mst[0][:], 0.0, ['M0'])
    mset('pool', BK[0][:], 0.0, ['BK0'])
    mset('pool', xT[:, :, 0:1], 0.0, ['xT%d' % dc for dc in range(16)])

    wst = xs
    for dc in range(16):
        st_ = wst[dc % 2]
        sn = 'xs0'
        dma(st_[:, 0:1824], w_in[dc * 128:(dc + 1) * 128, :], (), [sn], sn)
        cp('act', Wb[:, dc, 0:1280], st_[:, 0:1280], [sn], ['Wb'])
        cp('dve', Wb[:, dc, 1536:1824], st_[:, 1536:1824], [sn], ['Wb'])
        tt('dve', Wb[:, dc, 1280:1536], st_[:, 1280:1536], murv[:, 256:512], OP.mult, [sn, 'murv2'], ['Wb'])
        tt('pool', Wb[:, dc, 1824:2080], st_[:, 1280:1536], murv[:, 0:256], OP.mult, [sn, 'murv'], ['Wb'])

    cs_t = [sb(es, 'cs%d' % i, [128, 2, TS]) for i in range(2)]
    qb = sb(es, 'qb', [128, TS], BF16)
    Qt = sb(es, 'Qt', [128, 2, TS], BF16)
    t1 = sb(es, 't1', [128, TS])
    t2 = sb(es, 't2', [128, TS])
    gm = sb(es, 'gm', [128, NSUB, 16])
    m8 = sb(es, 'm8', [128, NSUB, 8])
    bia = sb(es, 'bia', [128, NSUB, 16])
    biaT = sb(es, 'biaT', [16, 2, TS], BF16)
    Pt = [sb(es, 'Pt%d' % i, [128, 256], BF16) for i in range(2)]
    rinv = sb(es, 'rinv', [128, 256])
    mixA = sb(es, 'mixA', [128, 2, TS], BF16)
    mixR = sb(es, 'mixR', [128, 2, TS], BF16)
    Vr = sb(es, 'Vr', [128, NSUB, 256])
    rawR = sb(es, 'rawR', [128, 2, TS + 1])
    rawK = sb(es, 'rawK', [128, 2, TS + 1])
    rawL = sb(es, 'rawL', [128, 3, TS + 1])
    for nm, bf in (('rawR', rawR), ('rawK', rawK), ('rawL', rawL)):
        mset('pool', bf[:], 0.0, [nm])
    NB = 12
    fb = [sb(es, 'fb%d' % i, [128, 2, TS]) for i in range(NB)]
    fbn = ['fb%d' % i for i in range(NB)]
    gtok = sb(es, 'gtok', [128, NSUB, 256])
    _mreal = [sb(es, 'mat%d' % i, [128, 512]) for i in range(4)]
    _alias = [0, 1, 2, 3, 6, 11]
    mats = [fb[k][:].rearrange("p a t -> p (a t)") for k in _alias] + [m_[:] for m_ in _mreal]
    matn = [fbn[k] for k in _alias] + ['mat%d' % i for i in range(4)]
    Zs = sb(es, 'Zs', [128, 256])
    Us = sb(es, 'Us', [128, 256])
    ysb = sb(es, 'ysb', [128, 256])
    ysq = sb(es, 'ysq', [128, 256])
    st4 = sb(es, 'st4', [128, 8, 4])
    rksc = sb(es, 'rksc', [128, 4])
    tmpM = sb(es, 'tmpM', [128, 128])

    SC = 1.0 / np.sqrt(128.0)

    _NT = int(os.environ.get('P1_TILES', NTILE))
    _PARTS = int(os.environ.get('P1_PARTS', 3))
    for tti in range(_NT):
        t0 = tti * TS
        xTn = ['xT%d' % dc for dc in range(16)]
        if tti > 0:
            cp('pool', xT[:, :, 0:1], xT[:, :, TS:TS + 1], xTn, xTn)
        for j in range(NSUB):
            dma(xs0[:], x[t0 + j * 128:t0 + (j + 1) * 128, :], (), ['xs0'], 'xs0')
            for q4 in range(4):
                b = nb()
                for k4 in range(4):
                    dc = q4 * 4 + k4
                    tr(ps[b][:, k4 * 128:(k4 + 1) * 128], xs0[:, dc * 128:(dc + 1) * 128], identF, ['xs0', 'CF'], ['ps%d' % b])
                cp('act' if q4 % 2 == 0 else 'dve', xT[:, q4 * 4:(q4 + 1) * 4, 1 + j * 128:1 + (j + 1) * 128],
                   ps[b][:].rearrange("p (c t) -> p c t", c=4), ['ps%d' % b], ['xT%d' % d_ for d_ in range(q4 * 4, q4 * 4 + 4)])
        csb = cs_t[tti % 2]
        csn = 'cs%d' % (tti % 2)
        dma(csb[:, 0, :], cosT[:, t0:t0 + TS], (), [csn + 'c'], csn)
        dma(csb[:, 1, :], sinT[:, t0:t0 + TS], (), [csn + 's'], csn)

        def proj_fm(col0, ncols, b):
            for dc in range(16):
                mm(ps[b][0:ncols, 0:TS], Wb[:, dc, col0:col0 + ncols], xT[:, dc, 1:TS + 1], dc == 0, dc == 15,
                   ['Wb', 'xT%d' % dc], ['ps%d' % b])

        for kind in range(2):
            for h in range(2):
                b = nb()
                proj_fm(kind * 256 + h * 128, 128, b)
                cp('act', qb[:], ps[b][:, 0:TS], ['ps%d' % b], ['qb'])
                b2 = nb()
                mm(ps[b2][:, 0:TS], rotB, qb[:], True, True, ['CB', 'qb'], ['ps%d' % b2])
                _H = os.environ.get('HYP', '')
                if _H == 'h1':
                    tt('dve', t1[:], ps[b][:, 0:TS], CF[:, 128:128 + TS], OP.mult, ['ps%d' % b, 'CF'], ['t1'])
                elif _H == 'h2':
                    tt('dve', t1[:], CF[:, 128:128 + TS], csb[:, 0, :], OP.mult, ['CF', csn], ['t1'])
                elif _H == 'h5':
                    tt('dve', t2[:], CF[:, 128:128 + TS], CF[:, 640:640 + TS], OP.mult, ['CF'], ['t2'])
                elif _H == 'h6':
                    mset('dve', t1[:], 0.0, ['t1'])
                elif _H == 'h7':
                    tt('dve', xs0[:, 0:TS], CF[:, 128:128 + TS], CF[:, 640:640 + TS], OP.mult, ['CF'], ['t2'])
                elif _H == 'h8':
                    P.op('dve', lambda e: e.memset(t1[:], 0.0), [csn + 'c', csn + 's'], ['t1'])
                elif _H == 'h9':
                    P.op('dve', lambda e: e.memset(t1[:], 0.0), ['ps%d' % b], ['t1'])
                elif _H == 'h3':
                    cp('dve', t1[:], ps[b][:, 0:TS], ['ps%d' % b], ['t1'])
                else:
                    tt('dve', t1[:], ps[b][:, 0:TS], csb[:, 0, :], OP.mult, ['ps%d' % b, csn + 'c', csn + 's'], ['t1'])
                tt('dve', t2[:], ps[b2][:, 0:TS], csb[:, 1, :], OP.mult, ['ps%d' % b2, csn + 'c', csn + 's'], ['t2'])
                if kind == 0:
                    tt('pool', Qt[:, h, :], t1[:], t2[:], OP.add, ['t1', 't2'], ['Qt'])
                else:
                    tt('pool', t1[:], t1[:], t2[:], OP.add, ['t1', 't2'], ['t1'])
                    cp('act', Kt[:, h, t0:t0 + TS], t1[:], ['t1'], ['Kt'])
                    P.op('dve', lambda e, h=h, tti=tti: e.tensor_reduce(
                        out=kms[:, h, tti:tti + 1], in_=t1[:].rearrange("p (a b) -> p a b", a=1),
                        axis=AX.X, op=OP.add), ['t1'], ['kms'])
        cp('dve', kmsB[:], kms[:], ['kms'], ['kmsB'])
        for j in range(NSUB):
            b = nb()
            for dc in range(16):
                mm(ps[b][:, 0:256], xT[:, dc, 1 + j * 128:1 + (j + 1) * 128], Wb[:, dc, 512:768], dc == 0, dc == 15,
                   ['Wb', 'xT%d' % dc], ['ps%d' % b])
            cp('act', Vtok[:, tti * NSUB + j, :], ps[b][:, 0:256], ['ps%d' % b], ['Vtok'])
        for j in range(NSUB):
            b = nb()
            for dc in range(16):
                mm(ps[b][:, 0:256], xT[:, dc, 1 + j * 128:1 + (j + 1) * 128], Wb[:, dc, 1280:1536], dc == 0, False,
                   ['Wb', 'xT%d' % dc], ['ps%d' % b])
                mm(ps[b][:, 0:256], xT[:, dc, j * 128:(j + 1) * 128], Wb[:, dc, 1824:2080], False, dc == 15,
                   ['Wb', 'xT%d' % dc], ['ps%d' % b])
            cp('dve', Vr[:, j, :], ps[b][:, 0:256], ['ps%d' % b], ['Vr'])
        groups = [(768, 128, rawR, 0, 'rawR'), (896, 128, rawR, 1, 'rawR'),
                  (1024, 128, rawK, 0, 'rawK'), (1152, 128, rawK, 1, 'rawK'),
                  (1536, 128, rawL, 0, 'rawL'), (1664, 128, rawL, 1, 'rawL'), (1792, 32, rawL, 2, 'rawL')]
        for (c0, ncl, bufr, slot, nm) in groups:
            b = nb()
            proj_fm(c0, ncl, b)
            cp('act', bufr[0:ncl, slot, 1:TS + 1], ps[b][0:ncl, 0:TS], ['ps%d' % b], [nm])

        for h in (range(2) if _PARTS & 1 else []):
            b = nb()
            for s_ in range(NSUB):
                mm(ps[b][:, s_ * 16:(s_ + 1) * 16], Qt[:, h, s_ * 128:(s_ + 1) * 128], kmsB[:, h, :], True, True,
                   ['Qt', 'kmsB'], ['ps%d' % b])
            for s_ in range(NSUB):
                cur = tti
                tt('dve', gm[:, s_, :], ps[b][:, s_ * 16:(s_ + 1) * 16], validb[:, cur * 16:(cur + 1) * 16], OP.add,
                   ['ps%d' % b, 'CF'], ['gm'])
            for s_ in range(NSUB):
                cur = tti
                P.op('dve', lambda e, s_=s_: e.max(out=m8[:, s_, :], in_=gm[:, s_, :]), ['gm'], ['m8'])
                ts('dve', bia[:, s_, :], gm[:, s_, :], m8[:, s_, 2:3], 1.0, OP.is_ge, OP.subtract, ['gm', 'm8'], ['bia'])
                stt('dve', bia[:, s_, :], bia[:, s_, :], 1.0e30, ownb[:, cur * 16:(cur + 1) * 16], OP.mult, OP.max,
                    ['bia', 'CF'], ['bia'])
                tt('dve', bia[:, s_, :], bia[:, s_, :], futb[:, cur * 16:(cur + 1) * 16], OP.min, ['bia', 'CF'], ['bia'])
            b2 = nb()
            for s_ in range(NSUB):
                tr(ps[b2][0:16, s_ * 128:(s_ + 1) * 128], bia[:, s_, :], identF, ['bia', 'CF'], ['ps%d' % b2])
            cp('act', biaT[:, h, :], ps[b2][0:16, 0:TS], ['ps%d' % b2], ['biaT'])
            for qbk in range(1):
                Qn = tti
                steps = [(n, kt) for n in range(Qn + 1) for kt in range(2)]
                for si, (n, kt) in enumerate(steps):
                    b = nb()
                    qs = slice(qbk * 256, (qbk + 1) * 256)
                    mm(ps[b][:, 0:256], Kt[:, h, n * 256 + kt * 128:n * 256 + (kt + 1) * 128], Qt[:, h, qs], True, False,
                       ['Kt', 'Qt'], ['ps%d' % b])
                    mm(ps[b][:, 0:256], onehotB[0:16, n * 128:(n + 1) * 128], biaT[:, h, qs], False, n != Qn,
                       ['CB', 'biaT'], ['ps%d' % b])
                    if n == Qn:
                        mm(ps[b][:, 0:256], identB, causB[:, kt * 256:(kt + 1) * 256], False, True, ['CB'], ['ps%d' % b])
                    pt = Pt[si % 2]
                    ptn = 'Pt%d' % (si % 2)
                    act(pt[:], ps[b][:, 0:256], AF.Exp, ['ps%d' % b], [ptn], scale=float(SC))
                    mm(ps[4][:, 0:256], Vtok[:, n * 2 + kt, h * 128:(h + 1) * 128], pt[:], si == 0, si == len(steps) - 1,
                       ['Vtok', ptn], ['ps4'])
                    mm(ps[5][:, 0:256], onesB, pt[:], si == 0, si == len(steps) - 1, ['CB', ptn], ['ps5'])
                P.op('dve', lambda e: e.reciprocal(out=rinv[:], in_=ps[5][:, 0:256]), ['ps5'], ['rinv'])
                tt('dve', mixA[:, h, qbk * 256:(qbk + 1) * 256], ps[4][:, 0:256], rinv[:], OP.mult, ['ps4', 'rinv'], ['mixA'])
        for h in (range(2) if _PARTS & 1 else []):
            dma(agin[h][:, t0:t0 + TS], mixA[:, h, :], ['mixA'], ['agin'], 'mixA')

        if not (_PARTS & 2):
            continue
        def shiftmix(bufr, slot, nm, mucol, o, on):
            ts('pool', t2[:], bufr[:, slot, 0:TS], pv[:, mucol:mucol + 1], None, OP.mult, None, [nm, 'pv'], ['t2'])
            stt('dve', o, bufr[:, slot, 1:TS + 1], pv[:, 17 + mucol:18 + mucol], t2[:], OP.mult, OP.add,
                [nm, 'pvb', 't2'], [on])
        for p in range(2):
            shiftmix(rawR, p, 'rawR', 0 + p, fb[0][:, p, :], fbn[0])
            shiftmix(rawK, p, 'rawK', 2 + p, fb[1][:, p, :], fbn[1])
        shiftmix(rawL, 0, 'rawL', 4, fb[2][:, 0, :], fbn[2])
        shiftmix(rawL, 1, 'rawL', 5, fb[3][:, 0, :], fbn[3])
        shiftmix(rawL, 2, 'rawL', 6, fb[3][:, 1, :], fbn[3])
        for nm, bf in (('rawR', rawR), ('rawK', rawK), ('rawL', rawL)):
            cp('pool', bf[:, :, 0:1], bf[:, :, TS:TS + 1], [nm], [nm])
        pr, pk = fb[0], fb[1]
        act(fb[2][0:64, 0, :], fb[2][0:64, 0, :], AF.Tanh, [fbn[2]], [fbn[2]])
        act(fb[3][:, 0, :], fb[3][:, 0, :], AF.Sigmoid, [fbn[3]], [fbn[3]])
        act(fb[3][0:32, 1, :], fb[3][0:32, 1, :], AF.Sigmoid, [fbn[3]], [fbn[3]])
        sig, alr = fb[4], fb[5]
        for p in range(2):
            b = nb()
            mm(ps[b][:, 0:TS], wupS[0:64, p * 128:(p + 1) * 128], fb[2][0:64, 0, :], True, True, ['wupS', fbn[2]], ['ps%d' % b])
            act(sig[:, p, :], ps[b][:, 0:TS], AF.Sigmoid, ['ps%d' % b, 'pv'], [fbn[4]], bias=pv[:, 7 + p:8 + p])
            b = nb()
            mm(ps[b][:, 0:TS], aupS[64:128, p * 128:(p + 1) * 128], fb[2][64:128, 0, :], True, True, ['aupS', fbn[2]], ['ps%d' % b])
            act(alr[:, p, :], ps[b][:, 0:TS], AF.Sigmoid, ['ps%d' % b, 'pv'], [fbn[5]], bias=pv[:, 9 + p:10 + p])
        for c in range(NSUB):
            b = nb()
            mm(ps[b][:, 0:256], fb[3][:, 0, c * 128:(c + 1) * 128], gupS[:, 0:256], True, False, [fbn[3], 'gupS', 'gupS2'], ['ps%d' % b])
            mm(ps[b][:, 0:256], fb[3][0:32, 1, c * 128:(c + 1) * 128], gupS[0:32, 256:512], False, True, [fbn[3], 'gupS', 'gupS2'], ['ps%d' % b])
            cp('act', gtok[:, c, :], ps[b][:, 0:256], ['ps%d' % b], ['gtok'])
        src, srcn = sig, fbn[4]
        pp = [(fb[6], fbn[6]), (fb[7], fbn[7])]
        k_ = 0
        dd = 1
        while dd < 128:
            dst, dstn = pp[k_ % 2]
            sv = src[:].rearrange("p a (c t) -> p a c t", c=NSUB)
            dv = dst[:].rearrange("p a (c t) -> p a c t", c=NSUB)
            for p in range(2):
                tt('dve', dv[:, p, :, dd:128], sv[:, p, :, dd:128], sv[:, p, :, 0:128 - dd], OP.add, [srcn], [dstn])
                cp('pool', dv[:, p, :, 0:dd], sv[:, p, :, 0:dd], [srcn], [dstn])
            src, srcn = dst, dstn
            k_ += 1
            dd *= 2
        cs_, csn_ = src, srcn
        other, othern = pp[k_ % 2]
        e_incl, e_inv, e_excl = fb[8], fb[9], fb[10]
        for p in range(2):
            tt('dve', other[:, p, :], cs_[:, p, :], sig[:, p, :], OP.subtract, [csn_, fbn[4]], [othern])
            act(e_incl[:, p, :], cs_[:, p, :], AF.Exp, [csn_], [fbn[8]], scale=-LD_SCALE)
            act(e_inv[:, p, :], cs_[:, p, :], AF.Exp, [csn_], [fbn[9]], scale=LD_SCALE)
            act(e_excl[:, p, :], other[:, p, :], AF.Exp, [othern], [fbn[10]], scale=-LD_SCALE)
        kraw, kk = fb[11], fb[6]
        sq = fb[7]
        for p in range(2):
            ts('dve', kraw[:, p, :], pk[:, p, :], pv[:, 11 + p:12 + p], None, OP.mult, None, [fbn[1], 'pv'], [fbn[11]])
            tt('pool', sq[:, p, :], kraw[:, p, :], kraw[:, p, :], OP.mult, [fbn[11]], [fbn[7]])
            b = nb()
            mm(ps[b][:, 0:TS], blockones, sq[:, p, :], True, True, ['CF', fbn[7]], ['ps%d' % b])
            ts('dve', sq[:, p, :], ps[b][:, 0:TS], 1.0e-24, None, OP.max, None, ['ps%d' % b], [fbn[7]])
            act(sq[:, p, :], sq[:, p, :], AF.Ln, [fbn[7]], [fbn[7]])
            act(sq[:, p, :], sq[:, p, :], AF.Exp, [fbn[7]], [fbn[7]], scale=-0.5)
            tt('dve', kk[:, p, :], kraw[:, p, :], sq[:, p, :], OP.mult, [fbn[11], fbn[7]], [fbn[6]])
        kp = fb[11]
        for p in range(2):
            ts('dve', sq[:, p, :], alr[:, p, :], pv[:, 13 + p:14 + p], pv2[:, p:p + 1], OP.mult, OP.add,
               [fbn[5], 'pv', 'pv2'], [fbn[7]])
            tt('dve', kp[:, p, :], pk[:, p, :], sq[:, p, :], OP.mult, [fbn[1], fbn[7]], [fbn[11]])
        At, Bt, Ktl, Rt, rkr = fb[10], fb[5], fb[9], fb[4], fb[7]
        for p in range(2):
            stt('dve', At[:, p, :], kk[:, p, :], -1.0, e_excl[:, p, :], OP.mult, OP.mult, [fbn[6], fbn[10]], [fbn[10]])
            tt('pool', Bt[:, p, :], kk[:, p, :], alr[:, p, :], OP.mult, [fbn[6], fbn[5]], [fbn[5]])
            tt('dve', Bt[:, p, :], Bt[:, p, :], e_inv[:, p, :], OP.mult, [fbn[5], fbn[9]], [fbn[5]])
            tt('pool', rkr[:, p, :], pr[:, p, :], kp[:, p, :], OP.mult, [fbn[0], fbn[11]], [fbn[7]])
            ts('pool', rkr[:, p, :], rkr[:, p, :], pv[:, 15 + p:16 + p], None, OP.mult, None, [fbn[7], 'pv'], [fbn[7]])
            tt('dve', Ktl[:, p, :], kp[:, p, :], e_inv[:, p, :], OP.mult, [fbn[11], fbn[9]], [fbn[9]])
            tt('dve', Rt[:, p, :], pr[:, p, :], e_incl[:, p, :], OP.mult, [fbn[0], fbn[8]], [fbn[4]])

        def fm(buf, h, c):
            e_, p_ = h % 2, h // 2
            return buf[e_ * 64:(e_ + 1) * 64, p_, c * 128:(c + 1) * 128]

        for c in range(NSUB):
            gc = tti * NSUB + c
            Mcur, Mn = Mst[gc % 2], 'M%d' % (gc % 2)
            Mnx, Mnn = Mst[(gc + 1) % 2], 'M%d' % ((gc + 1) % 2)
            bk, bkn = BK[0], 'BK0'
            def smat(L, Ln, R, Rn, mask, o, on):
                bp = (nb(), nb())
                for h in range(4):
                    e_, p_ = h % 2, h // 2
                    mm(ps[bp[e_]][:, p_ * 128:(p_ + 1) * 128], fm(L, h, c), fm(R, h, c), True, True, [Ln, Rn], ['ps%d' % bp[e_]])
                ov = o.rearrange("q (p e t) -> q p e t", p=2, e=2)
                for e_ in range(2):
                    tt('dve', ov[:, :, e_, :], ps[bp[e_]][:, 0:256].rearrange("q (p t) -> q p t", p=2),
                       mask[:, 0:256].rearrange("q (p t) -> q p t", p=2), OP.mult, ['ps%d' % bp[e_], 'CF'], [on])
            smat(Bt, fbn[5], At, fbn[10], SU4, mats[0], matn[0])
            smat(At, fbn[10], Bt, fbn[5], SL4, mats[1], matn[1])
            smat(Ktl, fbn[9], At, fbn[10], SU4, mats[2], matn[2])
            smat(Bt, fbn[5], Rt, fbn[4], UI4, mats[3], matn[3])
            smat(Ktl, fbn[9], Rt, fbn[4], UI4, mats[4], matn[4])
            Pc, Pn = mats[0], matn[0]
            Qc, Qn_ = mats[1], matn[1]
            Xc, Xn = mats[5], matn[5]
            idv = identF
            for h in range(4):
                tt('pool', Xc[:, h * 128:(h + 1) * 128], Pc[:, h * 128:(h + 1) * 128], idv, OP.add, [Pn, 'CF'], [Xn])
            free = [6, 7, 8, 9, 0, 1]
            fi = 0
            for lvl in range(6):
                qn_i = free[fi % 6]; fi += 1
                Q2, Q2n = mats[qn_i], matn[qn_i]
                b = nb()
                for h in range(4):
                    hs = slice(h * 128, (h + 1) * 128)
                    mm(ps[b][:, hs], Pc[:, hs], Qc[:, hs], True, True, [Pn, Qn_], ['ps%d' % b])
                cp('act', Q2[:], ps[b][:], ['ps%d' % b], [Q2n])
                if lvl < 5:
                    pn_i = free[fi % 6]; fi += 1
                    P2, P2n = mats[pn_i], matn[pn_i]
                    b = nb()
                    for h in range(4):
                        hs = slice(h * 128, (h + 1) * 128)
                        mm(ps[b][:, hs], Qc[:, hs], Pc[:, hs], True, True, [Pn, Qn_], ['ps%d' % b])
                    cp('dve', P2[:], ps[b][:], ['ps%d' % b], [P2n])
                b = nb()
                for h in range(4):
                    hs = slice(h * 128, (h + 1) * 128)
                    mm(ps[b][:, hs], Q2[:, hs], Xc[:, hs], True, True, [Q2n, Xn], ['ps%d' % b])
                xn_i = free[fi % 6]; fi += 1
                X2, X2n = mats[xn_i], matn[xn_i]
                tt('dve', X2[:], ps[b][:], Xc[:], OP.add, ['ps%d' % b, Xn], [X2n])
                Xc, Xn = X2, X2n
                Qc, Qn_ = Q2, Q2n
                if lvl < 5:
                    Pc, Pn = P2, P2n
            TT_, TTn = Xc, Xn
            bp = (nb(), nb())
            for kind, (src_b, src_n) in enumerate(((Bt, fbn[5]), (Ktl, fbn[9]))):
                for h in range(4):
                    e_, p_ = h % 2, h // 2
                    slot = kind * 2 + p_
                    tr(ps[bp[e_]][:, slot * 64:(slot + 1) * 64], fm(src_b, h, c), identF[e_ * 64:(e_ + 1) * 64, e_ * 64:(e_ + 1) * 64],
                       [src_n, 'CF'], ['ps%d' % bp[e_]])
            cp('act', bk[:, :, :, 0, 0:64], ps[bp[0]][:, 0:256].rearrange("p (k a j) -> p k a j", k=2, a=2), ['ps%d' % bp[0]], [bkn])
            cp('dve', bk[:, :, :, 1, 64:128], ps[bp[1]][:, 0:256].rearrange("p (k a j) -> p k a j", k=2, a=2), ['ps%d' % bp[1]], [bkn])
            bp = (nb(), nb())
            for h in range(4):
                e_, p_ = h % 2, h // 2
                mm(ps[bp[e_]][:, p_:p_ + 1], fm(rkr, h, c), CF[e_ * 64:(e_ + 1) * 64, 1664 + e_ * 64:1665 + e_ * 64], True, True,
                   [fbn[7], 'CF'], ['ps%d' % bp[e_]])
            rkv = rksc[:].rearrange("q (p e) -> q p e", p=2)
            cp('act', rkv[:, :, 0], ps[bp[0]][:, 0:2], ['ps%d' % bp[0]], ['rksc'])
            cp('dve', rkv[:, :, 1], ps[bp[1]][:, 0:2], ['ps%d' % bp[1]], ['rksc'])
            bp = (nb(), nb())
            for h in range(4):
                e_, p_ = h % 2, h // 2
                mm(ps[bp[e_]][:, p_ * 64:(p_ + 1) * 64], fm(At, h, c), Mcur[e_ * 64:(e_ + 1) * 64, p_, :], True, False,
                   [fbn[10], Mn], ['ps%d' % bp[e_]])
                mm(ps[bp[e_]][:, p_ * 64:(p_ + 1) * 64], mats[2][:, h * 128:(h + 1) * 128], Vr[:, c, h * 64:(h + 1) * 64], False, True,
                   [matn[2], 'Vr'], ['ps%d' % bp[e_]])
            zv = Zs[:].rearrange("q (p e i) -> q p e i", p=2, e=2)
            cp('act', zv[:, :, 0, :], ps[bp[0]][:, 0:128].rearrange("q (p i) -> q p i", p=2), ['ps%d' % bp[0]], ['Zs'])
            cp('dve', zv[:, :, 1, :], ps[bp[1]][:, 0:128].rearrange("q (p i) -> q p i", p=2), ['ps%d' % bp[1]], ['Zs'])
            bu = nb()
            for h in range(4):
                mm(ps[bu][:, h * 64:(h + 1) * 64], TT_[:, h * 128:(h + 1) * 128], Zs[:, h * 64:(h + 1) * 64], True, True,
                   [TTn, 'Zs'], ['ps%d' % bu])
            cp('dve', Us[:], ps[bu][:, 0:256], ['ps%d' % bu], ['Us'])
            bp = (nb(), nb())
            for h in range(4):
                e_, p_ = h % 2, h // 2
                hs = slice(h * 64, (h + 1) * 64)
                os_ = ps[bp[e_]][:, p_ * 64:(p_ + 1) * 64]
                mm(os_, fm(Rt, h, c), Mcur[e_ * 64:(e_ + 1) * 64, p_, :], True, False, [fbn[4], Mn], ['ps%d' % bp[e_]])
                mm(os_, mats[3][:, h * 128:(h + 1) * 128], Us[:, hs], False, False, [matn[3], 'Us'], ['ps%d' % bp[e_]])
                mm(os_, mats[4][:, h * 128:(h + 1) * 128], Vr[:, c, hs], False, True, [matn[4], 'Vr'], ['ps%d' % bp[e_]])
            yv = ysb[:].rearrange("q (p e i) -> q p e i", p=2, e=2)
            cp('act', yv[:, :, 0, :], ps[bp[0]][:, 0:128].rearrange("q (p i) -> q p i", p=2), ['ps%d' % bp[0]], ['ysb'])
            cp('dve', yv[:, :, 1, :], ps[bp[1]][:, 0:128].rearrange("q (p i) -> q p i", p=2), ['ps%d' % bp[1]], ['ysb'])
            bm = nb()
            for p_ in range(2):
                for e_ in range(2):
                    h = p_ * 2 + e_
                    hs = slice(h * 64, (h + 1) * 64)
                    mm(ps[bm][:, p_ * 64:(p_ + 1) * 64], bk[:, 0, p_, e_, :], Us[:, hs], e_ == 0, False, [bkn, 'Us'], ['ps%d' % bm])
                    mm(ps[bm][:, p_ * 64:(p_ + 1) * 64], bk[:, 1, p_, e_, :], Vr[:, c, hs], False, e_ == 1, [bkn, 'Vr'], ['ps%d' % bm])
            tt('dve', tmpM[:], ps[bm][:, 0:128], Mcur[:].rearrange("p a i -> p (a i)"), OP.add, ['ps%d' % bm, Mn], ['tmpM'])
            cl_ap = e_incl[:, :, c * 128 + 127:c * 128 + 128].to_broadcast([128, 2, 64])
            tt('dve', Mnx[:], tmpM[:].rearrange("p (a i) -> p a i", a=2), cl_ap, OP.mult, ['tmpM', fbn[8]], [Mnn])
            y3 = ysb[:].rearrange("p (h i) -> p h i", h=4)
            P.op('dve', lambda e, y3=y3: e.tensor_reduce(out=st4[:, 0, :], in_=y3, axis=AX.X, op=OP.add), ['ysb'], ['st4'])
            tt('pool', ysq[:], ysb[:], ysb[:], OP.mult, ['ysb'], ['ysq'])
            q3 = ysq[:].rearrange("p (h i) -> p h i", h=4)
            P.op('dve', lambda e, q3=q3: e.tensor_reduce(out=st4[:, 1, :], in_=q3, axis=AX.X, op=OP.add), ['ysq'], ['st4'])
            ts('dve', st4[:, 2, :], st4[:, 0, :], 1.0 / 64, None, OP.mult, None, ['st4'], ['st4'])
            tt('dve', st4[:, 3, :], st4[:, 2, :], st4[:, 2, :], OP.mult, ['st4'], ['st4'])
            stt('dve', st4[:, 4, :], st4[:, 1, :], 1.0 / 64, st4[:, 3, :], OP.mult, OP.subtract, ['st4'], ['st4'])
            ts('dve', st4[:, 5, :], st4[:, 4, :], 64e-5, None, OP.add, None, ['st4'], ['st4'])
            act(st4[:, 5, :], st4[:, 5, :], AF.Ln, ['st4'], ['st4'])
            act(st4[:, 5, :], st4[:, 5, :], AF.Exp, ['st4'], ['st4'], scale=-0.5)
            mean_b = st4[:, 2, :].unsqueeze(2).to_broadcast([128, 4, 64])
            rstd_b = st4[:, 5, :].unsqueeze(2).to_broadcast([128, 4, 64])
            tt('dve', y3, y3, mean_b, OP.subtract, ['ysb', 'st4'], ['ysb'])
            tt('dve', y3, y3, rstd_b, OP.mult, ['ysb', 'st4'], ['ysb'])
            tt('pool', ysb[:], ysb[:], rowb[:, 0:256], OP.mult, ['ysb', 'rowb'], ['ysb'])
            tt('pool', ysb[:], ysb[:], rowb[:, 256:512], OP.add, ['ysb', 'rowb'], ['ysb'])
            rk_b = rksc[:].unsqueeze(2).to_broadcast([128, 4, 64])
            v3 = Vr[:, c, :].rearrange("p (h i) -> p h i", h=4)
            tt('dve', q3, v3, rk_b, OP.mult, ['Vr', 'rksc'], ['ysq'])
            tt('dve', ysb[:], ysb[:], ysq[:], OP.add, ['ysb', 'ysq'], ['ysb'])
            tt('dve', ysb[:], ysb[:], gtok[:, c, :], OP.mult, ['ysb', 'gtok'], ['ysb'])
            bt_ = nb()
            for p_ in range(2):
                tr(ps[bt_][:, p_ * 128:(p_ + 1) * 128], ysb[:, p_ * 128:(p_ + 1) * 128], identF, ['ysb', 'CF'], ['ps%d' % bt_])
            cp('act', mixR[:, :, c * 128:(c + 1) * 128], ps[bt_][:, 0:256].rearrange("p (a t) -> p a t", a=2),
               ['ps%d' % bt_], ['mixR'])
        for p_ in range(2):
            dma(agin[2 + p_][:, t0:t0 + TS], mixR[:, p_, :], ['mixR'], ['agin'], 'mixR')

    P.flush()
    es.close()
    if dbg == 'p1':
        if os.environ.get('NOFINAL') != '1':
            P.final_wait()
        ges.close()
        return nc

    ccs = ges.enter_context(nc.semaphore('ccs'))
    with nc.Block() as blk:
        def body(g):
            for (semh, val, key) in P.fence:
                if val > 0:
                    g.wait_ge(semh, val)
            for f in range(4):
                g.collective_compute("AllGather", OP.bypass, replica_groups=[[0, 1, 2, 3], [4, 5, 6, 7]],
                                     ins=[agin[f]], outs=[agout[f]]).then_inc(ccs)
                g.wait_ge(ccs, f + 1)
        blk.gpsimd(body)
    P.fence = P.fence + [(ccs, 4, 'ccs')]

    es2 = ExitStack()
    acc = sb(es2, 'acc', [128, 8, D])
    x1T = sb(es2, 'x1T', [128, 16, 1024], BF16)
    G = sb(es2, 'G', [128, 8, 32])
    GT = sb(es2, 'GT', [32, 1024])
    esa = ExitStack()
    mixT = sb(esa, 'mixT', [128, 16, 1024], BF16)
    WoB = [sb(esa, 'WoB%d' % i, [128, 16, 256], BF16) for i in range(2)]
    wos = [sb(esa, 'wos%d' % i, [128, 4, 256]) for i in range(2)]
    xr = [sb(esa, 'xr%d' % i, [128, 256]) for i in range(2)]
    lnb = sb(esa, 'lnb', [128, 2, D])
    x1Tf = sb(esa, 'x1Tf', [128, 16, 128])
    rwS = sb(esa, 'rwS', [128, 16, 32])
    rbS = sb(esa, 'rbS', [128, 32])
    stl = sb(esa, 'stl', [128, 16])
    lg = sb(esa, 'lg', [128, 32])
    ex = sb(esa, 'ex', [128, 32])
    mk = sb(esa, 'mk', [128, 32])
    m8b = sb(esa, 'm8b', [128, 8])
    junk = sb(esa, 'junk', [128, D], BF16)

    def issue_mixT(eng):
        pid = eng.partition_id()
        off = (pid % 4) * 1024
        last = None
        for fc in range(16):
            last = eng.dma_start(out=mixT[:, fc, :], in_=agout[fc % 4][(fc // 4) * 128:(fc // 4 + 1) * 128, bass.ds(off, 1024)])
            if fc < 15:
                last.then_inc(P.dsem['mixT'], 16)
        return last
    P.dsem['mixT'] = ges.enter_context(nc.semaphore('d_mixT'))
    P.dcnt['mixT'] = 15 * 16
    P.op('sp', issue_mixT, (), ['mixT'], dma='mixT')
    dma(lnb[:, 0, :], lnv[:, 0:D].partition_broadcast(128), (), ['lnb'], 'lnb')
    dma(lnb[:, 1, :], lnv[:, D:2 * D].partition_broadcast(128), (), ['lnbB'], 'lnb')
    dma(rwS[:], router_w.rearrange("(c p) e -> p c e", p=128), (), ['rwS'], 'rwS')
    dma(rbS[:], router_b.partition_broadcast(128), (), ['rbS'], 'rbS')

    for mt in range(8):
        wb, wbn = WoB[mt % 2], 'WoB%d' % (mt % 2)
        for q4 in range(4):
            st_, sn = wos[q4 % 2], 'wos%d' % (q4 % 2)
            dma(st_[:], w_out[q4 * 512:(q4 + 1) * 512, mt * 256:(mt + 1) * 256].rearrange("(c p) m -> p c m", p=128),
                (), [sn], sn)
            cp('act' if q4 % 2 == 0 else 'pool', wb[:, q4 * 4:(q4 + 1) * 4, :], st_[:], [sn], [wbn])
        for tc in range(8):
            xb_, xn = xr[tc % 2], 'xr%d' % (tc % 2)
            dma(xb_[:], xres[tc * 128:(tc + 1) * 128, mt * 256:(mt + 1) * 256], (), [xn], xn)
            b = nb()
            for fc in range(16):
                mm(ps[b][:, 0:256], mixT[:, fc, tc * 128:(tc + 1) * 128], wb[:, fc, :], fc == 0, fc == 15,
                   ['mixT', wbn], ['ps%d' % b])
            stt('dve', acc[:, tc, mt * 256:(mt + 1) * 256], xb_[:], float(ALPHA), ps[b][:, 0:256], OP.mult, OP.add,
                [xn, 'ps%d' % b], ['acc%d' % tc])

    def layernorm(tc, gi, stl_, junk_, lnb_, lnbn):
        a = acc[:, tc, :]
        an = 'acc%d' % tc
        P.op('dve', lambda e: e.tensor_reduce(out=stl_[:, 0:1], in_=a, axis=AX.X, op=OP.add), [an], ['stl'])
        act(junk_[:], a, AF.Square, [an], ['junk', 'stl2'], accum=stl_[:, 1:2])
        ts('dve', stl_[:, 2:3], stl_[:, 0:1], 1.0 / D, None, OP.mult, None, ['stl'], ['stl'])
        tt('dve', stl_[:, 3:4], stl_[:, 2:3], stl_[:, 2:3], OP.mult, ['stl'], ['stl'])
        stt('dve', stl_[:, 4:5], stl_[:, 1:2], 1.0 / D, stl_[:, 3:4], OP.mult, OP.subtract, ['stl', 'stl2'], ['stl'])
        ts('dve', stl_[:, 5:6], stl_[:, 4:5], 1e-5, None, OP.add, None, ['stl'], ['stl'])
        act(stl_[:, 5:6], stl_[:, 5:6], AF.Ln, ['stl'], ['stl'])
        act(stl_[:, 5:6], stl_[:, 5:6], AF.Exp, ['stl'], ['stl'], scale=-0.5)
        ts('dve', a, a, stl_[:, 2:3], stl_[:, 5:6], OP.subtract, OP.mult, [an, 'stl'], [an])
        tt('pool', a, a, lnb_[:, gi, :], OP.mult, [an, lnbn, lnbn + 'B'], [an])
        tt('dve', a, a, lnb_[:, gi + 1, :], OP.add, [an, lnbn, lnbn + 'B'], [an])

    for tc in range(8):
        mset('dve', stl[:, 1:2], 0.0, ['stl2'])
        layernorm(tc, 0, stl, junk, lnb, 'lnb')
        an = 'acc%d' % tc
        for q4 in range(4):
            b = nb()
            for k4 in range(4):
                dc = q4 * 4 + k4
                tr(ps[b][:, k4 * 128:(k4 + 1) * 128], acc[:, tc, dc * 128:(dc + 1) * 128], identF, [an, 'CF'], ['ps%d' % b])
            cp('act', x1T[:, q4 * 4:(q4 + 1) * 4, tc * 128:(tc + 1) * 128],
               ps[b][:].rearrange("p (c t) -> p c t", c=4), ['ps%d' % b], ['x1T'])
            cp('dve', x1Tf[:, q4 * 4:(q4 + 1) * 4, :], ps[b][:].rearrange("p (c t) -> p c t", c=4), ['ps%d' % b], ['x1Tf'])
        b = nb()
        for dc in range(16):
            mm(ps[b][:, 0:32], x1Tf[:, dc, :], rwS[:, dc, :], dc == 0, dc == 15, ['x1Tf', 'rwS'], ['ps%d' % b])
        tt('dve', lg[:], ps[b][:, 0:32], rbS[:], OP.add, ['ps%d' % b, 'rbS'], ['lg'])
        P.op('dve', lambda e: e.max(out=m8b[:], in_=lg[:]), ['lg'], ['m8b'])
        ts('dve', mk[:], lg[:], m8b[:, 3:4], None, OP.is_ge, None, ['lg', 'm8b'], ['mk'])
        ts('dve', stl[:, 8:9], m8b[:, 0:1], -1.0, None, OP.mult, None, ['m8b'], ['stl3'])
        act(ex[:], lg[:], AF.Exp, ['lg', 'stl3'], ['ex'], bias=stl[:, 8:9])
        tt('dve', ex[:], ex[:], mk[:], OP.mult, ['ex', 'mk'], ['ex'])
        P.op('dve', lambda e: e.tensor_reduce(out=stl[:, 9:10], in_=ex[:], axis=AX.X, op=OP.add), ['ex'], ['stl3'])
        P.op('dve', lambda e: e.reciprocal(out=stl[:, 10:11], in_=stl[:, 9:10]), ['stl3'], ['stl3'])
        ts('dve', G[:, tc, :], ex[:], stl[:, 10:11], None, OP.mult, None, ['ex', 'stl3'], ['G'])
        b = nb()
        tr(ps[b][0:32, 0:128], G[:, tc, :], identF, ['G', 'CF'], ['ps%d' % b])
        cp('act', GT[:, tc * 128:(tc + 1) * 128], ps[b][0:32, 0:128], ['ps%d' % b], ['GT'])
        ts('pool', acc[:, tc, :], acc[:, tc, :], float(ALPHA), None, OP.mult, None, [an], [an])
    if dbg == 'p2a':
        for tc in range(8):
            dma(out[tc * 128:(tc + 1) * 128, :], acc[:, tc, :], ['acc%d' % tc], ['out%d' % tc], 'out')
    P.flush()
    esa.close()
    if dbg == 'p2a':
        P.final_wait()
        es2.close()
        ges.close()
        return nc

    esb = ExitStack()
    AT = sb(esb, 'AT', [128, 16, 1024], BF16)
    wsg = [sb(esb, 'wsg%d' % i, [128, 4, 256]) for i in range(3)]
    WgB = [sb(esb, 'WgB%d' % i, [128, 16, 256], BF16) for i in range(2)]
    WdB = [sb(esb, 'WdB%d' % i, [128, 16, 256], BF16) for i in range(2)]
    bguS = sb(esb, 'bguS', [128, 1024])
    bdS = sb(esb, 'bdS', [32, 512])
    gt_ = [sb(esb, 'gt%d' % i, [128, 512]) for i in range(1)]
    sg_ = [sb(esb, 'sg%d' % i, [128, 512]) for i in range(1)]
    ut_ = [sb(esb, 'ut%d' % i, [128, 512]) for i in range(1)]
    dma(bguS[:], bgu_p, (), ['bguS'], 'bguS')
    for mq in range(4):
        dma(bdS[:], b_down[:, mq * 512:(mq + 1) * 512], (), ['bdS'], 'bdS')
        for tc in range(8):
            b = nb()
            mm(ps[b][:], GT[:, tc * 128:(tc + 1) * 128], bdS[:], True, True, ['GT', 'bdS'], ['ps%d' % b])
            tt('dve', acc[:, tc, mq * 512:(mq + 1) * 512], acc[:, tc, mq * 512:(mq + 1) * 512], ps[b][:], OP.add,
                   ['ps%d' % b, 'acc%d' % tc], ['acc%d' % tc])
    sgi = 0
    it = 0
    for e_ in range(32):
        for fc in range(16):
            wg, wgn = WgB[it % 2], 'WgB%d' % (it % 2)
            for q4 in range(4):
                for half in range(2):
                    st_, sn = wsg[sgi % 3], 'wsg%d' % (sgi % 3)
                    c0 = half * 2048 + fc * 128
                    dma(st_[:, :, 0:128], w_gu[e_, q4 * 512:(q4 + 1) * 512, c0:c0 + 128].rearrange("(c p) m -> p c m", p=128),
                        (), [sn], sn)
                    cp('act' if sgi % 2 == 0 else 'pool', wg[:, q4 * 4:(q4 + 1) * 4, half * 128:(half + 1) * 128],
                       st_[:, :, 0:128], [sn], [wgn])
                    sgi += 1
            for th in range(2):
                tsl = slice(th * 512, (th + 1) * 512)
                bg = nb()
                for dc in range(16):
                    mm(ps[bg][:], wg[:, dc, 0:128], x1T[:, dc, tsl], dc == 0, dc == 15, [wgn, 'x1T'], ['ps%d' % bg])
                bu = nb()
                for dc in range(16):
                    mm(ps[bu][:], wg[:, dc, 128:256], x1T[:, dc, tsl], dc == 0, dc == 15, [wgn, 'x1T'], ['ps%d' % bu])
                k2 = 0
                g_, gn = gt_[k2], 'gt%d' % k2
                s__, sn_ = sg_[k2], 'sg%d' % k2
                u_, un = ut_[k2], 'ut%d' % k2
                bcol_g = bguS[:, e_ * 32 + fc:e_ * 32 + fc + 1]
                bcol_u = bguS[:, e_ * 32 + 16 + fc:e_ * 32 + 16 + fc + 1]
                ts('dve', g_[:], ps[bg][:], bcol_g, 7.0, OP.add, OP.min, ['ps%d' % bg, 'bguS'], [gn])
                act(s__[:], g_[:], AF.Sigmoid, [gn], [sn_], scale=1.702)
                tt('pool', g_[:], g_[:], s__[:], OP.mult, [gn, sn_], [gn])
                ts('dve', u_[:], ps[bu][:], bcol_u, 7.0, OP.add, OP.min, ['ps%d' % bu, 'bguS'], [un])
                ts('pool', u_[:], u_[:], -7.0, 1.0, OP.max, OP.add, [un], [un])
                tt('dve', AT[:, fc, tsl], u_[:], g_[:], OP.mult, [un, gn], ['AT'])
            it += 1
        for mt in range(8):
            wd, wdn = WdB[mt % 2], 'WdB%d' % (mt % 2)
            for q4 in range(4):
                st_, sn = wsg[sgi % 3], 'wsg%d' % (sgi % 3)
                dma(st_[:], w_down[e_, q4 * 512:(q4 + 1) * 512, mt * 256:(mt + 1) * 256].rearrange("(c p) m -> p c m", p=128),
                    (), [sn], sn)
                cp('act' if sgi % 2 == 0 else 'pool', wd[:, q4 * 4:(q4 + 1) * 4, :], st_[:], [sn], [wdn])
                sgi += 1
            for tc in range(8):
                b = nb()
                for fc in range(16):
                    mm(ps[b][:, 0:256], AT[:, fc, tc * 128:(tc + 1) * 128], wd[:, fc, :], fc == 0, fc == 15,
                       ['AT', wdn], ['ps%d' % b])
                msl = slice(mt * 256, (mt + 1) * 256)
                stt('dve', acc[:, tc, msl], ps[b][:, 0:256], G[:, tc, e_:e_ + 1], acc[:, tc, msl], OP.mult, OP.add,
                    ['ps%d' % b, 'G', 'acc%d' % tc], ['acc%d' % tc])
    P.flush()
    esb.close()

    esc = ExitStack()
    lnb2 = sb(esc, 'lnb2', [128, 2, D])
    stl2 = sb(esc, 'stl2', [128, 16])
    junk2 = sb(esc, 'junk2', [128, D], BF16)
    dma(lnb2[:, 0, :], lnv[:, 2 * D:3 * D].partition_broadcast(128), (), ['lnb2'], 'lnb2')
    dma(lnb2[:, 1, :], lnv[:, 3 * D:4 * D].partition_broadcast(128), (), ['lnb2B'], 'lnb2')
    for tc in range(8):
        mset('dve', stl2[:, 1:2], 0.0, ['stl2'])
        layernorm(tc, 0, stl2, junk2, lnb2, 'lnb2')
        dma(out[tc * 128:(tc + 1) * 128, :], acc[:, tc, :], ['acc%d' % tc], ['out%d' % tc], 'out')
    P.flush()
    P.final_wait()
    esc.close()
    es2.close()
    ges.close()
    return nc


def _consts():
    inv = (10000.0 ** (-np.arange(0, 128, 2, dtype=np.float32) / 128.0)).astype(np.float32)
    ang = np.arange(T, dtype=np.float32)[:, None] * inv[None, :]
    cos = np.concatenate([np.cos(ang), np.cos(ang)], -1).T.astype(np.float32)
    sin = np.concatenate([np.sin(ang), np.sin(ang)], -1).T.astype(np.float32)
    cf = np.zeros((128, 128 + 3 * 512 + 128 + 3 * 256), np.float32)
    cf[:, 0:128] = np.eye(128)
    i = np.arange(128)
    su = (i[:, None] < i[None, :]).astype(np.float32)
    sl = (i[:, None] > i[None, :]).astype(np.float32)
    ui = (i[:, None] <= i[None, :]).astype(np.float32)
    cf[:, 128:640] = np.tile(su, (1, 4))
    cf[:, 640:1152] = np.tile(sl, (1, 4))
    cf[:, 1152:1664] = np.tile(ui, (1, 4))
    bo = np.zeros((128, 128), np.float32)
    bo[0:64, 0:64] = 1
    bo[64:, 64:] = 1
    cf[:, 1664:1792] = bo
    n = np.arange(16)
    for cur in range(16):
        cf[:, 1792 + cur * 16:1792 + (cur + 1) * 16] = np.where(n < cur, 0.0, NEG)[None, :]
        cf[:, 2048 + cur * 16:2048 + (cur + 1) * 16] = np.where(n == cur, 0.0, NEG)[None, :]
        cf[:, 2304 + cur * 16:2304 + (cur + 1) * 16] = np.where(n > cur, NEG, 0.0)[None, :]
    cb = np.zeros((128, 128 + 128 + 128 + 512 + 2048), np.float32)
    cb[:, 0:128] = np.eye(128)
    rot = np.zeros((128, 128), np.float32)
    for dp in range(64):
        rot[dp + 64, dp] = -1.0
    for dp in range(64, 128):
        rot[dp - 64, dp] = 1.0
    cb[:, 128:256] = rot
    cb[:, 256:384] = 1.0
    q = np.arange(256)
    for kt in range(2):
        kk = kt * 128 + i
        cb[:, 384 + kt * 256:384 + (kt + 1) * 256] = np.where(kk[:, None] <= q[None, :], 0.0, NEG)
    for nn in range(16):
        cb[nn, 896 + nn * 128:896 + (nn + 1) * 128] = 1.0
    return cos, sin, cf, cb.astype(ml_dtypes.bfloat16)


_NC = None


def _prep(inputs, p1only=False, p2a=False):
    f = lambda a: np.ascontiguousarray(np.asarray(a, dtype=np.float32))
    x = f(inputs['x'])
    w_in = f(inputs['w_in'])[0]
    mu = f(inputs['mu_shift'])[0]
    cos, sin, cf, cb = _consts()
    if p1only:
        shared = dict(cosT=cos, sinT=sin, cf32=cf, cbf=cb)
    else:
        w_out = f(inputs['w_out'])[0]
        perm = np.concatenate([np.concatenate([np.arange(256 * g, 256 * g + 256), 1024 + np.arange(256 * g, 256 * g + 256)])
                               for g in range(4)])
        w_out_p = np.ascontiguousarray(w_out[perm])
        lnv = np.concatenate([f(inputs['ln1_g'])[0], f(inputs['ln1_b'])[0], f(inputs['ln2_g'])[0], f(inputs['ln2_b'])[0]])[None, :]
        if p2a:
            shared = dict(w_out_p=w_out_p, lnv=np.ascontiguousarray(lnv), router_w=f(inputs['router_w'])[0],
                          router_b=f(inputs['router_b']), cosT=cos, sinT=sin, cf32=cf, cbf=cb)
        else:
            b_gu = f(inputs['b_gu'])[0]
            bgu_p = np.ascontiguousarray(b_gu.reshape(32, 32, 128).transpose(2, 0, 1).reshape(128, 1024))
            shared = dict(w_out_p=w_out_p, lnv=np.ascontiguousarray(lnv), router_w=f(inputs['router_w'])[0],
                          router_b=f(inputs['router_b']), w_gu=f(inputs['w_gu'])[0], bgu_p=bgu_p,
                          w_down=f(inputs['w_down'])[0], b_down=f(inputs['b_down'])[0],
                          cosT=cos, sinT=sin, cf32=cf, cbf=cb)
    maps = []
    for c in range(8):
        b, g = c // 4, c % 4
        sl = slice(256 * g, 256 * g + 256)
        cols = np.concatenate([np.arange(256 * g, 256 * g + 256), 1024 + np.arange(256 * g, 256 * g + 256),
                               2048 + np.arange(256 * g, 256 * g + 256), 3072 + np.arange(256 * g, 256 * g + 256),
                               4096 + np.arange(256 * g, 256 * g + 256), 5120 + np.arange(256 * g, 256 * g + 256),
                               np.arange(6144, 6432)])
        pvec = np.zeros((128, 20), np.float32)

        def two(v):
            return v[sl].reshape(2, 128).T
        pvec[:, 0:2] = two(mu[0:1024])
        pvec[:, 2:4] = two(mu[1024:2048])
        pvec[:, 4] = mu[3072:3200]
        pvec[:, 5] = mu[3200:3328]
        pvec[0:32, 6] = mu[3328:3360]
        pvec[:, 7:9] = two(f(inputs['w0'])[0])
        pvec[:, 9:11] = two(f(inputs['a0'])[0])
        pvec[:, 11:13] = two(f(inputs['k_k'])[0])
        pvec[:, 13:15] = two(f(inputs['k_a'])[0])
        pvec[:, 15:17] = two(f(inputs['r_k'])[0].reshape(-1))
        rowv = np.concatenate([f(inputs['lnx_g'])[0][sl], f(inputs['lnx_b'])[0][sl]])[None, :]
        m = dict(shared)
        if not p1only:
            m['xres'] = np.ascontiguousarray(x[b, 1024 * g:1024 * (g + 1)])
        m.update(x=x[b],
                 w_in_c=np.ascontiguousarray(w_in[:, cols]),
                 mu_rv=np.ascontiguousarray(mu[2048:3072][sl][None, :]), pvec=pvec, rowv=np.ascontiguousarray(rowv),
                 w_up_c=np.ascontiguousarray(f(inputs['w_up'])[0][:, sl]),
                 a_up_c=np.ascontiguousarray(f(inputs['a_up'])[0][:, sl]),
                 g_up_c=np.ascontiguousarray(f(inputs['g_up'])[0][:, sl]))
        maps.append(m)
    return maps


def kernel(**inputs):
    global _NC
    if _NC is None:
        _NC = build()
    maps = _prep(inputs)
    res = run_bass_kernel_spmd(_NC, maps, core_ids=list(range(8)))
    outs = [np.asarray(r['out'], dtype=np.float32) for r in res.results]
    return np.stack(outs, 0).reshape(2, 4096, 2048)
```

```python
import os
import numpy as np
import ml_dtypes
from contextlib import ExitStack
import concourse.bass as bass
import concourse.mybir as mybir
from concourse.bass_utils import run_bass_kernel_spmd

F32 = mybir.dt.float32
BF16 = mybir.dt.bfloat16
AF = mybir.ActivationFunctionType
OP = mybir.AluOpType
AX = mybir.AxisListType

T = 4096
TS = 256
NSUB = 2
NTILE = 16
D = 2048
NEG = -1.0e30
ALPHA = 2.0 ** 0.25
LD_SCALE = float(np.exp(-0.5))
ENG = ['pe', 'act', 'dve', 'pool', 'sp']
BLK = {'pe': 'tensor', 'act': 'scalar', 'dve': 'vector', 'pool': 'gpsimd', 'sp': 'sync'}


class Prog:
    def __init__(s, nc, es):
        s.nc = nc
        s.es = es
        s.ops = []
        s.lastw = {}
        s.readers = {}
        s.esem = {e: es.enter_context(nc.semaphore('s_' + e)) for e in ENG}
        s.ecnt = {e: 0 for e in ENG}
        s.dsem = {}
        s.dcnt = {}
        s.flushed = 0
        s.waited = {e: {} for e in ENG}
        s.fence = []
        s.rr = 0
        s.dhist = {}

    def op(s, eng, fn, r=(), w=(), dma=None):
        idx = len(s.ops)
        if idx >= int(os.environ.get('MAXOPS', 10 ** 9)):
            return -1
        deps = set()
        for x in r:
            if x in s.lastw:
                deps.add(s.lastw[x])
            if x.startswith('ps'):
                for rd in s.readers.get(x, ()):
                    if s.ops[rd]['eng'] != eng:
                        deps.add(rd)
        for x in w:
            rds = s.readers.get(x, ())
            if x in s.lastw:
                lw = s.ops[s.lastw[x]]
                if not (dma is not None and lw['dma'] == dma and len(rds) > 0):
                    deps.add(s.lastw[x])
            deps.update(rds)
        for x in r:
            s.readers.setdefault(x, []).append(idx)
        for x in w:
            s.lastw[x] = idx
            s.readers[x] = []
        if dma is not None and dma not in s.dsem:
            s.dsem[dma] = s.es.enter_context(s.nc.semaphore('d_' + dma))
            s.dcnt[dma] = 0
        s.ops.append(dict(eng=eng, fn=fn, deps=deps, dma=dma, needed=False, idx=idx))
        return idx

    def flush(s):
        nc = s.nc
        lo = s.flushed
        ops = s.ops[lo:]
        for o in ops:
            best = {}
            for d in o['deps']:
                if d < lo:
                    continue
                Dd = s.ops[d]
                if Dd['dma'] is not None:
                    continue
                if Dd['eng'] == 'pe' and o['eng'] == 'pe' and o['dma'] is None:
                    continue
                if d > best.get(Dd['eng'], -1):
                    best[Dd['eng']] = d
            for d in best.values():
                s.ops[d]['needed'] = True
        last = {}
        for i, o in enumerate(ops):
            if o['dma'] is None:
                last[o['eng']] = i
        for e, i in last.items():
            ops[i]['needed'] = True
        for gi, o in enumerate(ops, lo):
            if o['dma'] is not None:
                k = o['dma']
                s.dcnt[k] += 16
                o['sv'] = (s.dsem[k], s.dcnt[k], 'd' + k)
                s.dhist.setdefault(k, []).append((gi, s.dcnt[k]))
            elif o['needed']:
                s.ecnt[o['eng']] += 1
                o['sv'] = (s.esem[o['eng']], s.ecnt[o['eng']], 'e' + o['eng'])
        with nc.Block() as blk:
            for e in ENG:
                myops = [o for o in ops if o['eng'] == e]

                def body(eng, myops=myops, e=e):
                    waited = s.waited[e]
                    for (semh, val, key) in s.fence:
                        if val > 0 and waited.get(key, 0) < val:
                            eng.wait_ge(semh, val)
                            waited[key] = val
                    for o in myops:
                        oi = o['idx']
                        for d in sorted(o['deps']):
                            if d < lo:
                                continue
                            Dd = s.ops[d]
                            if 'sv' not in Dd:
                                continue
                            if Dd['dma'] is None and Dd['eng'] == 'pe' and e == 'pe' and o['dma'] is None:
                                continue
                            semh, val, key = Dd['sv']
                            if Dd['dma'] is not None:
                                for (gi2, v2) in s.dhist[Dd['dma']]:
                                    if gi2 < oi and v2 > val:
                                        val = v2
                            if waited.get(key, 0) < val:
                                eng.wait_ge(semh, val)
                                waited[key] = val
                        ins = o['fn'](eng)
                        if 'sv' in o:
                            ins.then_inc(o['sv'][0], 16 if o['dma'] is not None else 1)
                getattr(blk, BLK[e])(body)
        s.fence = [(s.esem[e], s.ecnt[e], 'e' + e) for e in ENG] + \
                  [(h, s.dcnt[k], 'd' + k) for k, h in s.dsem.items()]
        s.flushed = len(s.ops)

    def final_wait(s):
        with s.nc.Block() as blk:
            def body(eng):
                for (semh, val, key) in s.fence:
                    if val > 0:
                        eng.wait_ge(semh, val)
            blk.sync(body)


def build(dbg=None):
    nc = bass.Bass("TRN2", target_bir_lowering=False)

    def din(name, shape, dt=F32):
        return nc.dram_tensor(name, list(shape), dt, kind="ExternalInput").ap()

    x = din('x', [T, D])
    if dbg != 'p1':
        xres = din('xres', [1024, D])
    w_in = din('w_in_c', [D, 1824])
    mu_rv = din('mu_rv', [1, 256])
    pvec = din('pvec', [128, 20])
    rowv = din('rowv', [1, 512])
    w_up = din('w_up_c', [64, 256])
    a_up = din('a_up_c', [64, 256])
    g_up = din('g_up_c', [160, 256])
    if dbg != 'p1':
        w_out = din('w_out_p', [D, D])
        lnv = din('lnv', [1, 4 * D])
        router_w = din('router_w', [D, 32])
        router_b = din('router_b', [1, 32])
    if dbg is None:
        w_gu = din('w_gu', [32, D, 4096])
        bgu_p = din('bgu_p', [128, 32 * 32])
        w_down = din('w_down', [32, D, D])
        b_down = din('b_down', [32, D])
    cosT = din('cosT', [128, T])
    sinT = din('sinT', [128, T])
    cf32 = din('cf32', [128, 128 + 3 * 512 + 128 + 3 * 256])
    cbf = din('cbf', [128, 128 + 128 + 128 + 512 + 16 * 128], BF16)
    tokoff = None
    if dbg == 'p1':
        _dbgt = nc.dram_tensor('dbg', [512, T], BF16, kind="ExternalOutput").ap()
        agin = [_dbgt[f * 128:(f + 1) * 128, :] for f in range(4)]
    else:
        out = nc.dram_tensor('dbg2' if dbg == 'p2a' else 'out', [1024, D], F32, kind="ExternalOutput").ap()
        agin = [nc.dram_tensor('agin%d' % f, [128, T], BF16, kind="Internal").ap() for f in range(4)]
        agout = [nc.dram_tensor('agout%d' % f, [4 * 128, T], BF16, kind="Internal").ap() for f in range(4)]

    ges = ExitStack()
    P = Prog(nc, ges)
    ps = [ges.enter_context(nc.psum_tensor('ps%d' % i, [128, 512], F32)) for i in range(8)]
    rot_banks = [0, 1, 2, 3, 6, 7]

    def nb():
        P.rr = (P.rr + 1) % len(rot_banks)
        return rot_banks[P.rr]

    def sb(es, name, shape, dt=F32):
        return es.enter_context(nc.sbuf_tensor(name, list(shape), dt))

    def mm(o, l, r_, st, sp_, rd, wr):
        P.op('pe', lambda e: e.matmul(o, lhsT=l, rhs=r_, start=st, stop=sp_), rd, wr)

    def tr(o, i, idn, rd, wr):
        P.op('pe', lambda e: e.transpose(o, i, idn), rd, wr)

    def act(o, i, f, rd, wr, bias=None, scale=None, accum=None):
        kw = {}
        if bias is not None:
            kw['bias'] = bias
        if scale is not None:
            kw['scale'] = scale
        if accum is not None:
            kw['accum_out'] = accum
        P.op('act', lambda e: e.activation(out=o, in_=i, func=f, **kw), rd, wr)

    def tt(eng, o, a, b, op, rd, wr):
        P.op(eng, lambda e: e.tensor_tensor(out=o, in0=a, in1=b, op=op), rd, wr)

    def ts(eng, o, a, s1, s2, op0, op1, rd, wr):
        if s2 is None:
            P.op(eng, lambda e: e.tensor_scalar(out=o, in0=a, scalar1=s1, scalar2=None, op0=op0), rd, wr)
        else:
            P.op(eng, lambda e: e.tensor_scalar(out=o, in0=a, scalar1=s1, scalar2=s2, op0=op0, op1=op1), rd, wr)

    def stt(eng, o, a, sc, b, op0, op1, rd, wr):
        P.op(eng, lambda e: e.scalar_tensor_tensor(out=o, in0=a, scalar=sc, in1=b, op0=op0, op1=op1), rd, wr)

    def cp(eng, o, i, rd, wr):
        if eng == 'act':
            P.op('act', lambda e: e.copy(out=o, in_=i), rd, wr)
        else:
            P.op(eng, lambda e: e.tensor_copy(out=o, in_=i), rd, wr)

    def dma(o, i, rd, wr, key, q='sp'):
        P.op(q, lambda e: e.dma_start(out=o, in_=i), rd, wr, dma=key)

    def mset(eng, o, v, wr):
        P.op(eng, lambda e: e.memset(o, v), (), wr)

    CF = sb(ges, 'CF', [128, 128 + 3 * 512 + 128 + 3 * 256])
    CB = sb(ges, 'CB', [128, 128 + 128 + 128 + 512 + 16 * 128], BF16)
    dma(CF[:], cf32, (), ['CF'], 'c0')
    dma(CB[:], cbf, (), ['CB'], 'c1')
    identF = CF[:, 0:128]
    SU4 = CF[:, 128:640]
    SL4 = CF[:, 640:1152]
    UI4 = CF[:, 1152:1664]
    blockones = CF[:, 1664:1792]
    validb = CF[:, 1792:2048]
    ownb = CF[:, 2048:2304]
    futb = CF[:, 2304:2560]
    identB = CB[:, 0:128]
    rotB = CB[:, 128:256]
    onesB = CB[:, 256:384]
    causB = CB[:, 384:896]
    onehotB = CB[:, 896:896 + 2048]

    es = ExitStack()
    Wb = sb(es, 'Wb', [128, 16, 2080], BF16)
    Kt = sb(es, 'Kt', [128, 2, T], BF16)
    Vtok = sb(es, 'Vtok', [128, 32, 256], BF16)
    kms = sb(es, 'kms', [128, 2, 16])
    kmsB = sb(es, 'kmsB', [128, 2, 16], BF16)
    pv = sb(es, 'pv', [128, 24])
    rowb = sb(es, 'rowb', [128, 512])
    murv = sb(es, 'murv', [128, 512])
    wupS = sb(es, 'wupS', [128, 256])
    aupS = sb(es, 'aupS', [128, 256])
    gupS = sb(es, 'gupS', [128, 512])
    xs0 = sb(es, 'xs0', [128, D])
    xs = [xs0, xs0]
    xsn = ['xs0', 'xs0']
    xT = sb(es, 'xT', [128, 16, TS + 1], BF16)
    Mst = [sb(es, 'M%d' % i, [128, 2, 64]) for i in range(2)]
    _bk0 = sb(es, 'BK0', [128, 2, 2, 2, 128])
    BK = [_bk0, _bk0]

    dma(pv[:, 0:20], pvec, (), ['pv'], 'c2')
    dma(rowb[:], rowv.partition_broadcast(128), (), ['rowb'], 'c3')
    dma(murv[:, 0:256], mu_rv.partition_broadcast(128), (), ['murv'], 'c4')
    dma(wupS[0:64, :], w_up, (), ['wupS'], 'c5')
    dma(aupS[64:128, :], a_up, (), ['aupS'], 'c6')
    dma(gupS[:, 0:256], g_up[0:128, :], (), ['gupS'], 'c7')
    dma(gupS[0:32, 256:512], g_up[128:160, :], (), ['gupS2'], 'c7')
    pv2 = sb(es, 'pv2', [128, 16])
    ts('dve', pv[:, 17:24], pv[:, 0:7], -1.0, 1.0, OP.mult, OP.add, ['pv'], ['pvb'])
    ts('dve', pv2[:, 0:2], pv[:, 13:15], -1.0, 1.0, OP.mult, OP.add, ['pv'], ['pv2'])
    ts('dve', murv[:, 256:512], murv[:, 0:256], -1.0, 1.0, OP.mult, OP.add, ['murv'], ['murv2'])
    mset('dve', kms[:], 0.0, ['kms'])
    mset('dve', Mst[0][:], 0.0, ['M0'])
    mset('pool', BK[0][:], 0.0, ['BK0'])
    mset('pool', xT[:, :, 0:1], 0.0, ['xT%d' % dc for dc in range(16)])

    wst = xs
    for dc in range(16):
        st_ = wst[dc % 2]
        sn = 'xs0'
        dma(st_[:, 0:1824], w_in[dc * 128:(dc + 1) * 128, :], (), [sn], sn)
        cp('act', Wb[:, dc, 0:1280], st_[:, 0:1280], [sn], ['Wb'])
        cp('dve', Wb[:, dc, 1536:1824], st_[:, 1536:1824], [sn], ['Wb'])
        tt('dve', Wb[:, dc, 1280:1536], st_[:, 1280:1536], murv[:, 256:512], OP.mult, [sn, 'murv2'], ['Wb'])
        tt('pool', Wb[:, dc, 1824:2080], st_[:, 1280:1536], murv[:, 0:256], OP.mult, [sn, 'murv'], ['Wb'])

    cs_t = [sb(es, 'cs%d' % i, [128, 2, TS]) for i in range(2)]
    qb = sb(es, 'qb', [128, TS], BF16)
    Qt = sb(es, 'Qt', [128, 2, TS], BF16)
    t1 = sb(es, 't1', [128, TS])
    t2 = sb(es, 't2', [128, TS])
    gm = sb(es, 'gm', [128, NSUB, 16])
    m8 = sb(es, 'm8', [128, NSUB, 8])
    bia = sb(es, 'bia', [128, NSUB, 16])
    biaT = sb(es, 'biaT', [16, 2, TS], BF16)
    Pt = [sb(es, 'Pt%d' % i, [128, 256], BF16) for i in range(2)]
    rinv = sb(es, 'rinv', [128, 256])
    mixA = sb(es, 'mixA', [128, 2, TS], BF16)
    mixR = sb(es, 'mixR', [128, 2, TS], BF16)
    Vr = sb(es, 'Vr', [128, NSUB, 256])
    rawR = sb(es, 'rawR', [128, 2, TS + 1])
    rawK = sb(es, 'rawK', [128, 2, TS + 1])
    rawL = sb(es, 'rawL', [128, 3, TS + 1])
    for nm, bf in (('rawR', rawR), ('rawK', rawK), ('rawL', rawL)):
        mset('pool', bf[:], 0.0, [nm])
    NB = 12
    fb = [sb(es, 'fb%d' % i, [128, 2, TS]) for i in range(NB)]
    fbn = ['fb%d' % i for i in range(NB)]
    gtok = sb(es, 'gtok', [128, NSUB, 256])
    _mreal = [sb(es, 'mat%d' % i, [128, 512]) for i in range(4)]
    _alias = [0, 1, 2, 3, 6, 11]
    mats = [fb[k][:].rearrange("p a t -> p (a t)") for k in _alias] + [m_[:] for m_ in _mreal]
    matn = [fbn[k] for k in _alias] + ['mat%d' % i for i in range(4)]
    Zs = sb(es, 'Zs', [128, 256])
    Us = sb(es, 'Us', [128, 256])
    ysb = sb(es, 'ysb', [128, 256])
    ysq = sb(es, 'ysq', [128, 256])
    st4 = sb(es, 'st4', [128, 8, 4])
    rksc = sb(es, 'rksc', [128, 4])
    tmpM = sb(es, 'tmpM', [128, 128])

    SC = 1.0 / np.sqrt(128.0)

    _NT = int(os.environ.get('P1_TILES', NTILE))
    _PARTS = int(os.environ.get('P1_PARTS', 3))
    for tti in range(_NT):
        t0 = tti * TS
        xTn = ['xT%d' % dc for dc in range(16)]
        if tti > 0:
            cp('pool', xT[:, :, 0:1], xT[:, :, TS:TS + 1], xTn, xTn)
        for j in range(NSUB):
            dma(xs0[:], x[t0 + j * 128:t0 + (j + 1) * 128, :], (), ['xs0'], 'xs0')
            for q4 in range(4):
                b = nb()
                for k4 in range(4):
                    dc = q4 * 4 + k4
                    tr(ps[b][:, k4 * 128:(k4 + 1) * 128], xs0[:, dc * 128:(dc + 1) * 128], identF, ['xs0', 'CF'], ['ps%d' % b])
                cp('act' if q4 % 2 == 0 else 'dve', xT[:, q4 * 4:(q4 + 1) * 4, 1 + j * 128:1 + (j + 1) * 128],
                   ps[b][:].rearrange("p (c t) -> p c t", c=4), ['ps%d' % b], ['xT%d' % d_ for d_ in range(q4 * 4, q4 * 4 + 4)])
        csb = cs_t[tti % 2]
        csn = 'cs%d' % (tti % 2)
        dma(csb[:, 0, :], cosT[:, t0:t0 + TS], (), [csn + 'c'], csn)
        dma(csb[:, 1, :], sinT[:, t0:t0 + TS], (), [csn + 's'], csn)

        def proj_fm(col0, ncols, b):
            for dc in range(16):
                mm(ps[b][0:ncols, 0:TS], Wb[:, dc, col0:col0 + ncols], xT[:, dc, 1:TS + 1], dc == 0, dc == 15,
                   ['Wb', 'xT%d' % dc], ['ps%d' % b])

        for kind in range(2):
            for h in range(2):
                b = nb()
                proj_fm(kind * 256 + h * 128, 128, b)
                cp('act', qb[:], ps[b][:, 0:TS], ['ps%d' % b], ['qb'])
                b2 = nb()
                mm(ps[b2][:, 0:TS], rotB, qb[:], True, True, ['CB', 'qb'], ['ps%d' % b2])
                _H = os.environ.get('HYP', '')
                if _H == 'h1':
                    tt('dve', t1[:], ps[b][:, 0:TS], CF[:, 128:128 + TS], OP.mult, ['ps%d' % b, 'CF'], ['t1'])
                elif _H == 'h2':
                    tt('dve', t1[:], CF[:, 128:128 + TS], csb[:, 0, :], OP.mult, ['CF', csn], ['t1'])
                elif _H == 'h5':
                    tt('dve', t2[:], CF[:, 128:128 + TS], CF[:, 640:640 + TS], OP.mult, ['CF'], ['t2'])
                elif _H == 'h6':
                    mset('dve', t1[:], 0.0, ['t1'])
                elif _H == 'h7':
                    tt('dve', xs0[:, 0:TS], CF[:, 128:128 + TS], CF[:, 640:640 + TS], OP.mult, ['CF'], ['t2'])
                elif _H == 'h8':
                    P.op('dve', lambda e: e.memset(t1[:], 0.0), [csn + 'c', csn + 's'], ['t1'])
                elif _H == 'h9':
                    P.op('dve', lambda e: e.memset(t1[:], 0.0), ['ps%d' % b], ['t1'])
                elif _H == 'h3':
                    cp('dve', t1[:], ps[b][:, 0:TS], ['ps%d' % b], ['t1'])
                else:
                    tt('dve', t1[:], ps[b][:, 0:TS], csb[:, 0, :], OP.mult, ['ps%d' % b, csn + 'c', csn + 's'], ['t1'])
                tt('dve', t2[:], ps[b2][:, 0:TS], csb[:, 1, :], OP.mult, ['ps%d' % b2, csn + 'c', csn + 's'], ['t2'])
                if kind == 0:
                    tt('pool', Qt[:, h, :], t1[:], t2[:], OP.add, ['t1', 't2'], ['Qt'])
                else:
                    tt('pool', t1[:], t1[:], t2[:], OP.add, ['t1', 't2'], ['t1'])
                    cp('act', Kt[:, h, t0:t0 + TS], t1[:], ['t1'], ['Kt'])
                    P.op('dve', lambda e, h=h, tti=tti: e.tensor_reduce(
                        out=kms[:, h, tti:tti + 1], in_=t1[:].rearrange("p (a b) -> p a b", a=1),
                        axis=AX.X, op=OP.add), ['t1'], ['kms'])
        cp('dve', kmsB[:], kms[:], ['kms'], ['kmsB'])
        for j in range(NSUB):
            b = nb()
            for dc in range(16):
                mm(ps[b][:, 0:256], xT[:, dc, 1 + j * 128:1 + (j + 1) * 128], Wb[:, dc, 512:768], dc == 0, dc == 15,
                   ['Wb', 'xT%d' % dc], ['ps%d' % b])
            cp('act', Vtok[:, tti * NSUB + j, :], ps[b][:, 0:256], ['ps%d' % b], ['Vtok'])
        for j in range(NSUB):
            b = nb()
            for dc in range(16):
                mm(ps[b][:, 0:256], xT[:, dc, 1 + j * 128:1 + (j + 1) * 128], Wb[:, dc, 1280:1536], dc == 0, False,
                   ['Wb', 'xT%d' % dc], ['ps%d' % b])
                mm(ps[b][:, 0:256], xT[:, dc, j * 128:(j + 1) * 128], Wb[:, dc, 1824:2080], False, dc == 15,
                   ['Wb', 'xT%d' % dc], ['ps%d' % b])
            cp('dve', Vr[:, j, :], ps[b][:, 0:256], ['ps%d' % b], ['Vr'])
        groups = [(768, 128, rawR, 0, 'rawR'), (896, 128, rawR, 1, 'rawR'),
                  (1024, 128, rawK, 0, 'rawK'), (1152, 128, rawK, 1, 'rawK'),
                  (1536, 128, rawL, 0, 'rawL'), (1664, 128, rawL, 1, 'rawL'), (1792, 32, rawL, 2, 'rawL')]
        for (c0, ncl, bufr, slot, nm) in groups:
            b = nb()
            proj_fm(c0, ncl, b)
            cp('act', bufr[0:ncl, slot, 1:TS + 1], ps[b][0:ncl, 0:TS], ['ps%d' % b], [nm])

        for h in (range(2) if _PARTS & 1 else []):
            b = nb()
            for s_ in range(NSUB):
                mm(ps[b][:, s_ * 16:(s_ + 1) * 16], Qt[:, h, s_ * 128:(s_ + 1) * 128], kmsB[:, h, :], True, True,
                   ['Qt', 'kmsB'], ['ps%d' % b])
            for s_ in range(NSUB):
                cur = tti
                tt('dve', gm[:, s_, :], ps[b][:, s_ * 16:(s_ + 1) * 16], validb[:, cur * 16:(cur + 1) * 16], OP.add,
                   ['ps%d' % b, 'CF'], ['gm'])
            for s_ in range(NSUB):
                cur = tti
                P.op('dve', lambda e, s_=s_: e.max(out=m8[:, s_, :], in_=gm[:, s_, :]), ['gm'], ['m8'])
                ts('dve', bia[:, s_, :], gm[:, s_, :], m8[:, s_, 2:3], 1.0, OP.is_ge, OP.subtract, ['gm', 'm8'], ['bia'])
                stt('dve', bia[:, s_, :], bia[:, s_, :], 1.0e30, ownb[:, cur * 16:(cur + 1) * 16], OP.mult, OP.max,
                    ['bia', 'CF'], ['bia'])
                tt('dve', bia[:, s_, :], bia[:, s_, :], futb[:, cur * 16:(cur + 1) * 16], OP.min, ['bia', 'CF'], ['bia'])
            b2 = nb()
            for s_ in range(NSUB):
                tr(ps[b2][0:16, s_ * 128:(s_ + 1) * 128], bia[:, s_, :], identF, ['bia', 'CF'], ['ps%d' % b2])
            cp('act', biaT[:, h, :], ps[b2][0:16, 0:TS], ['ps%d' % b2], ['biaT'])
            for qbk in range(1):
                Qn = tti
                steps = [(n, kt) for n in range(Qn + 1) for kt in range(2)]
                for si, (n, kt) in enumerate(steps):
                    b = nb()
                    qs = slice(qbk * 256, (qbk + 1) * 256)
                    mm(ps[b][:, 0:256], Kt[:, h, n * 256 + kt * 128:n * 256 + (kt + 1) * 128], Qt[:, h, qs], True, False,
                       ['Kt', 'Qt'], ['ps%d' % b])
                    mm(ps[b][:, 0:256], onehotB[0:16, n * 128:(n + 1) * 128], biaT[:, h, qs], False, n != Qn,
                       ['CB', 'biaT'], ['ps%d' % b])
                    if n == Qn:
                        mm(ps[b][:, 0:256], identB, causB[:, kt * 256:(kt + 1) * 256], False, True, ['CB'], ['ps%d' % b])
                    pt = Pt[si % 2]
                    ptn = 'Pt%d' % (si % 2)
                    act(pt[:], ps[b][:, 0:256], AF.Exp, ['ps%d' % b], [ptn], scale=float(SC))
                    mm(ps[4][:, 0:256], Vtok[:, n * 2 + kt, h * 128:(h + 1) * 128], pt[:], si == 0, si == len(steps) - 1,
                       ['Vtok', ptn], ['ps4'])
                    mm(ps[5][:, 0:256], onesB, pt[:], si == 0, si == len(steps) - 1, ['CB', ptn], ['ps5'])
                P.op('dve', lambda e: e.reciprocal(out=rinv[:], in_=ps[5][:, 0:256]), ['ps5'], ['rinv'])
                tt('dve', mixA[:, h, qbk * 256:(qbk + 1) * 256], ps[4][:, 0:256], rinv[:], OP.mult, ['ps4', 'rinv'], ['mixA'])
        for h in (range(2) if _PARTS & 1 else []):
            dma(agin[h][:, t0:t0 + TS], mixA[:, h, :], ['mixA'], ['agin'], 'mixA')

        if not (_PARTS & 2):
            continue
        def shiftmix(bufr, slot, nm, mucol, o, on):
            ts('pool', t2[:], bufr[:, slot, 0:TS], pv[:, mucol:mucol + 1], None, OP.mult, None, [nm, 'pv'], ['t2'])
            stt('dve', o, bufr[:, slot, 1:TS + 1], pv[:, 17 + mucol:18 + mucol], t2[:], OP.mult, OP.add,
                [nm, 'pvb', 't2'], [on])
        for p in range(2):
            shiftmix(rawR, p, 'rawR', 0 + p, fb[0][:, p, :], fbn[0])
            shiftmix(rawK, p, 'rawK', 2 + p, fb[1][:, p, :], fbn[1])
        shiftmix(rawL, 0, 'rawL', 4, fb[2][:, 0, :], fbn[2])
        shiftmix(rawL, 1, 'rawL', 5, fb[3][:, 0, :], fbn[3])
        shiftmix(rawL, 2, 'rawL', 6, fb[3][:, 1, :], fbn[3])
        for nm, bf in (('rawR', rawR), ('rawK', rawK), ('rawL', rawL)):
            cp('pool', bf[:, :, 0:1], bf[:, :, TS:TS + 1], [nm], [nm])
        pr, pk = fb[0], fb[1]
        act(fb[2][0:64, 0, :], fb[2][0:64, 0, :], AF.Tanh, [fbn[2]], [fbn[2]])
        act(fb[3][:, 0, :], fb[3][:, 0, :], AF.Sigmoid, [fbn[3]], [fbn[3]])
        act(fb[3][0:32, 1, :], fb[3][0:32, 1, :], AF.Sigmoid, [fbn[3]], [fbn[3]])
        sig, alr = fb[4], fb[5]
        for p in range(2):
            b = nb()
            mm(ps[b][:, 0:TS], wupS[0:64, p * 128:(p + 1) * 128], fb[2][0:64, 0, :], True, True, ['wupS', fbn[2]], ['ps%d' % b])
            act(sig[:, p, :], ps[b][:, 0:TS], AF.Sigmoid, ['ps%d' % b, 'pv'], [fbn[4]], bias=pv[:, 7 + p:8 + p])
            b = nb()
            mm(ps[b][:, 0:TS], aupS[64:128, p * 128:(p + 1) * 128], fb[2][64:128, 0, :], True, True, ['aupS', fbn[2]], ['ps%d' % b])
            act(alr[:, p, :], ps[b][:, 0:TS], AF.Sigmoid, ['ps%d' % b, 'pv'], [fbn[5]], bias=pv[:, 9 + p:10 + p])
        for c in range(NSUB):
            b = nb()
            mm(ps[b][:, 0:256], fb[3][:, 0, c * 128:(c + 1) * 128], gupS[:, 0:256], True, False, [fbn[3], 'gupS', 'gupS2'], ['ps%d' % b])
            mm(ps[b][:, 0:256], fb[3][0:32, 1, c * 128:(c + 1) * 128], gupS[0:32, 256:512], False, True, [fbn[3], 'gupS', 'gupS2'], ['ps%d' % b])
            cp('act', gtok[:, c, :], ps[b][:, 0:256], ['ps%d' % b], ['gtok'])
        src, srcn = sig, fbn[4]
        pp = [(fb[6], fbn[6]), (fb[7], fbn[7])]
        k_ = 0
        dd = 1
        while dd < 128:
            dst, dstn = pp[k_ % 2]
            sv = src[:].rearrange("p a (c t) -> p a c t", c=NSUB)
            dv = dst[:].rearrange("p a (c t) -> p a c t", c=NSUB)
            for p in range(2):
                tt('dve', dv[:, p, :, dd:128], sv[:, p, :, dd:128], sv[:, p, :, 0:128 - dd], OP.add, [srcn], [dstn])
                cp('pool', dv[:, p, :, 0:dd], sv[:, p, :, 0:dd], [srcn], [dstn])
            src, srcn = dst, dstn
            k_ += 1
            dd *= 2
        cs_, csn_ = src, srcn
        other, othern = pp[k_ % 2]
        e_incl, e_inv, e_excl = fb[8], fb[9], fb[10]
        for p in range(2):
            tt('dve', other[:, p, :], cs_[:, p, :], sig[:, p, :], OP.subtract, [csn_, fbn[4]], [othern])
            act(e_incl[:, p, :], cs_[:, p, :], AF.Exp, [csn_], [fbn[8]], scale=-LD_SCALE)
            act(e_inv[:, p, :], cs_[:, p, :], AF.Exp, [csn_], [fbn[9]], scale=LD_SCALE)
            act(e_excl[:, p, :], other[:, p, :], AF.Exp, [othern], [fbn[10]], scale=-LD_SCALE)
        kraw, kk = fb[11], fb[6]
        sq = fb[7]
        for p in range(2):
            ts('dve', kraw[:, p, :], pk[:, p, :], pv[:, 11 + p:12 + p], None, OP.mult, None, [fbn[1], 'pv'], [fbn[11]])
            tt('pool', sq[:, p, :], kraw[:, p, :], kraw[:, p, :], OP.mult, [fbn[11]], [fbn[7]])
            b = nb()
            mm(ps[b][:, 0:TS], blockones, sq[:, p, :], True, True, ['CF', fbn[7]], ['ps%d' % b])
            ts('dve', sq[:, p, :], ps[b][:, 0:TS], 1.0e-24, None, OP.max, None, ['ps%d' % b], [fbn[7]])
            act(sq[:, p, :], sq[:, p, :], AF.Ln, [fbn[7]], [fbn[7]])
            act(sq[:, p, :], sq[:, p, :], AF.Exp, [fbn[7]], [fbn[7]], scale=-0.5)
            tt('dve', kk[:, p, :], kraw[:, p, :], sq[:, p, :], OP.mult, [fbn[11], fbn[7]], [fbn[6]])
        kp = fb[11]
        for p in range(2):
            ts('dve', sq[:, p, :], alr[:, p, :], pv[:, 13 + p:14 + p], pv2[:, p:p + 1], OP.mult, OP.add,
               [fbn[5], 'pv', 'pv2'], [fbn[7]])
            tt('dve', kp[:, p, :], pk[:, p, :], sq[:, p, :], OP.mult, [fbn[1], fbn[7]], [fbn[11]])
        At, Bt, Ktl, Rt, rkr = fb[10], fb[5], fb[9], fb[4], fb[7]
        for p in range(2):
            stt('dve', At[:, p, :], kk[:, p, :], -1.0, e_excl[:, p, :], OP.mult, OP.mult, [fbn[6], fbn[10]], [fbn[10]])
            tt('pool', Bt[:, p, :], kk[:, p, :], alr[:, p, :], OP.mult, [fbn[6], fbn[5]], [fbn[5]])
            tt('dve', Bt[:, p, :], Bt[:, p, :], e_inv[:, p, :], OP.mult, [fbn[5], fbn[9]], [fbn[5]])
            tt('pool', rkr[:, p, :], pr[:, p, :], kp[:, p, :], OP.mult, [fbn[0], fbn[11]], [fbn[7]])
            ts('pool', rkr[:, p, :], rkr[:, p, :], pv[:, 15 + p:16 + p], None, OP.mult, None, [fbn[7], 'pv'], [fbn[7]])
            tt('dve', Ktl[:, p, :], kp[:, p, :], e_inv[:, p, :], OP.mult, [fbn[11], fbn[9]], [fbn[9]])
            tt('dve', Rt[:, p, :], pr[:, p, :], e_incl[:, p, :], OP.mult, [fbn[0], fbn[8]], [fbn[4]])

        def fm(buf, h, c):
            e_, p_ = h % 2, h // 2
            return buf[e_ * 64:(e_ + 1) * 64, p_, c * 128:(c + 1) * 128]

        for c in range(NSUB):
            gc = tti * NSUB + c
            Mcur, Mn = Mst[gc % 2], 'M%d' % (gc % 2)
            Mnx, Mnn = Mst[(gc + 1) % 2], 'M%d' % ((gc + 1) % 2)
            bk, bkn = BK[0], 'BK0'
            def smat(L, Ln, R, Rn, mask, o, on):
                bp = (nb(), nb())
                for h in range(4):
                    e_, p_ = h % 2, h // 2
                    mm(ps[bp[e_]][:, p_ * 128:(p_ + 1) * 128], fm(L, h, c), fm(R, h, c), True, True, [Ln, Rn], ['ps%d' % bp[e_]])
                ov = o.rearrange("q (p e t) -> q p e t", p=2, e=2)
                for e_ in range(2):
                    tt('dve', ov[:, :, e_, :], ps[bp[e_]][:, 0:256].rearrange("q (p t) -> q p t", p=2),
                       mask[:, 0:256].rearrange("q (p t) -> q p t", p=2), OP.mult, ['ps%d' % bp[e_], 'CF'], [on])
            smat(Bt, fbn[5], At, fbn[10], SU4, mats[0], matn[0])
            smat(At, fbn[10], Bt, fbn[5], SL4, mats[1], matn[1])
            smat(Ktl, fbn[9], At, fbn[10], SU4, mats[2], matn[2])
            smat(Bt, fbn[5], Rt, fbn[4], UI4, mats[3], matn[3])
            smat(Ktl, fbn[9], Rt, fbn[4], UI4, mats[4], matn[4])
            Pc, Pn = mats[0], matn[0]
            Qc, Qn_ = mats[1], matn[1]
            Xc, Xn = mats[5], matn[5]
            idv = identF
            for h in range(4):
                tt('pool', Xc[:, h * 128:(h + 1) * 128], Pc[:, h * 128:(h + 1) * 128], idv, OP.add, [Pn, 'CF'], [Xn])
            free = [6, 7, 8, 9, 0, 1]
            fi = 0
            for lvl in range(6):
                qn_i = free[fi % 6]; fi += 1
                Q2, Q2n = mats[qn_i], matn[qn_i]
                b = nb()
                for h in range(4):
                    hs = slice(h * 128, (h + 1) * 128)
                    mm(ps[b][:, hs], Pc[:, hs], Qc[:, hs], True, True, [Pn, Qn_], ['ps%d' % b])
                cp('act', Q2[:], ps[b][:], ['ps%d' % b], [Q2n])
                if lvl < 5:
                    pn_i = free[fi % 6]; fi += 1
                    P2, P2n = mats[pn_i], matn[pn_i]
                    b = nb()
                    for h in range(4):
                        hs = slice(h * 128, (h + 1) * 128)
                        mm(ps[b][:, hs], Qc[:, hs], Pc[:, hs], True, True, [Pn, Qn_], ['ps%d' % b])
                    cp('dve', P2[:], ps[b][:], ['ps%d' % b], [P2n])
                b = nb()
                for h in range(4):
                    hs = slice(h * 128, (h + 1) * 128)
                    mm(ps[b][:, hs], Q2[:, hs], Xc[:, hs], True, True, [Q2n, Xn], ['ps%d' % b])
                xn_i = free[fi % 6]; fi += 1
                X2, X2n = mats[xn_i], matn[xn_i]
                tt('dve', X2[:], ps[b][:], Xc[:], OP.add, ['ps%d' % b, Xn], [X2n])
                Xc, Xn = X2, X2n
                Qc, Qn_ = Q2, Q2n
                if lvl < 5:
                    Pc, Pn = P2, P2n
            TT_, TTn = Xc, Xn
            bp = (nb(), nb())
            for kind, (src_b, src_n) in enumerate(((Bt, fbn[5]), (Ktl, fbn[9]))):
                for h in range(4):
                    e_, p_ = h % 2, h // 2
                    slot = kind * 2 + p_
                    tr(ps[bp[e_]][:, slot * 64:(slot + 1) * 64], fm(src_b, h, c), identF[e_ * 64:(e_ + 1) * 64, e_ * 64:(e_ + 1) * 64],
                       [src_n, 'CF'], ['ps%d' % bp[e_]])
            cp('act', bk[:, :, :, 0, 0:64], ps[bp[0]][:, 0:256].rearrange("p (k a j) -> p k a j", k=2, a=2), ['ps%d' % bp[0]], [bkn])
            cp('dve', bk[:, :, :, 1, 64:128], ps[bp[1]][:, 0:256].rearrange("p (k a j) -> p k a j", k=2, a=2), ['ps%d' % bp[1]], [bkn])
            bp = (nb(), nb())
            for h in range(4):
                e_, p_ = h % 2, h // 2
                mm(ps[bp[e_]][:, p_:p_ + 1], fm(rkr, h, c), CF[e_ * 64:(e_ + 1) * 64, 1664 + e_ * 64:1665 + e_ * 64], True, True,
                   [fbn[7], 'CF'], ['ps%d' % bp[e_]])
            rkv = rksc[:].rearrange("q (p e) -> q p e", p=2)
            cp('act', rkv[:, :, 0], ps[bp[0]][:, 0:2], ['ps%d' % bp[0]], ['rksc'])
            cp('dve', rkv[:, :, 1], ps[bp[1]][:, 0:2], ['ps%d' % bp[1]], ['rksc'])
            bp = (nb(), nb())
            for h in range(4):
                e_, p_ = h % 2, h // 2
                mm(ps[bp[e_]][:, p_ * 64:(p_ + 1) * 64], fm(At, h, c), Mcur[e_ * 64:(e_ + 1) * 64, p_, :], True, False,
                   [fbn[10], Mn], ['ps%d' % bp[e_]])
                mm(ps[bp[e_]][:, p_ * 64:(p_ + 1) * 64], mats[2][:, h * 128:(h + 1) * 128], Vr[:, c, h * 64:(h + 1) * 64], False, True,
                   [matn[2], 'Vr'], ['ps%d' % bp[e_]])
            zv = Zs[:].rearrange("q (p e i) -> q p e i", p=2, e=2)
            cp('act', zv[:, :, 0, :], ps[bp[0]][:, 0:128].rearrange("q (p i) -> q p i", p=2), ['ps%d' % bp[0]], ['Zs'])
            cp('dve', zv[:, :, 1, :], ps[bp[1]][:, 0:128].rearrange("q (p i) -> q p i", p=2), ['ps%d' % bp[1]], ['Zs'])
            bu = nb()
            for h in range(4):
                mm(ps[bu][:, h * 64:(h + 1) * 64], TT_[:, h * 128:(h + 1) * 128], Zs[:, h * 64:(h + 1) * 64], True, True,
                   [TTn, 'Zs'], ['ps%d' % bu])
            cp('dve', Us[:], ps[bu][:, 0:256], ['ps%d' % bu], ['Us'])
            bp = (nb(), nb())
            for h in range(4):
                e_, p_ = h % 2, h // 2
                hs = slice(h * 64, (h + 1) * 64)
                os_ = ps[bp[e_]][:, p_ * 64:(p_ + 1) * 64]
                mm(os_, fm(Rt, h, c), Mcur[e_ * 64:(e_ + 1) * 64, p_, :], True, False, [fbn[4], Mn], ['ps%d' % bp[e_]])
                mm(os_, mats[3][:, h * 128:(h + 1) * 128], Us[:, hs], False, False, [matn[3], 'Us'], ['ps%d' % bp[e_]])
                mm(os_, mats[4][:, h * 128:(h + 1) * 128], Vr[:, c, hs], False, True, [matn[4], 'Vr'], ['ps%d' % bp[e_]])
            yv = ysb[:].rearrange("q (p e i) -> q p e i", p=2, e=2)
            cp('act', yv[:, :, 0, :], ps[bp[0]][:, 0:128].rearrange("q (p i) -> q p i", p=2), ['ps%d' % bp[0]], ['ysb'])
            cp('dve', yv[:, :, 1, :], ps[bp[1]][:, 0:128].rearrange("q (p i) -> q p i", p=2), ['ps%d' % bp[1]], ['ysb'])
            bm = nb()
            for p_ in range(2):
                for e_ in range(2):
                    h = p_ * 2 + e_
                    hs = slice(h * 64, (h + 1) * 64)
                    mm(ps[bm][:, p_ * 64:(p_ + 1) * 64], bk[:, 0, p_, e_, :], Us[:, hs], e_ == 0, False, [bkn, 'Us'], ['ps%d' % bm])
                    mm(ps[bm][:, p_ * 64:(p_ + 1) * 64], bk[:, 1, p_, e_, :], Vr[:, c, hs], False, e_ == 1, [bkn, 'Vr'], ['ps%d' % bm])
            tt('dve', tmpM[:], ps[bm][:, 0:128], Mcur[:].rearrange("p a i -> p (a i)"), OP.add, ['ps%d' % bm, Mn], ['tmpM'])
            cl_ap = e_incl[:, :, c * 128 + 127:c * 128 + 128].to_broadcast([128, 2, 64])
            tt('dve', Mnx[:], tmpM[:].rearrange("p (a i) -> p a i", a=2), cl_ap, OP.mult, ['tmpM', fbn[8]], [Mnn])
            y3 = ysb[:].rearrange("p (h i) -> p h i", h=4)
            P.op('dve', lambda e, y3=y3: e.tensor_reduce(out=st4[:, 0, :], in_=y3, axis=AX.X, op=OP.add), ['ysb'], ['st4'])
            tt('pool', ysq[:], ysb[:], ysb[:], OP.mult, ['ysb'], ['ysq'])
            q3 = ysq[:].rearrange("p (h i) -> p h i", h=4)
            P.op('dve', lambda e, q3=q3: e.tensor_reduce(out=st4[:, 1, :], in_=q3, axis=AX.X, op=OP.add), ['ysq'], ['st4'])
            ts('dve', st4[:, 2, :], st4[:, 0, :], 1.0 / 64, None, OP.mult, None, ['st4'], ['st4'])
            tt('dve', st4[:, 3, :], st4[:, 2, :], st4[:, 2, :], OP.mult, ['st4'], ['st4'])
            stt('dve', st4[:, 4, :], st4[:, 1, :], 1.0 / 64, st4[:, 3, :], OP.mult, OP.subtract, ['st4'], ['st4'])
            ts('dve', st4[:, 5, :], st4[:, 4, :], 64e-5, None, OP.add, None, ['st4'], ['st4'])
            act(st4[:, 5, :], st4[:, 5, :], AF.Ln, ['st4'], ['st4'])
            act(st4[:, 5, :], st4[:, 5, :], AF.Exp, ['st4'], ['st4'], scale=-0.5)
            mean_b = st4[:, 2, :].unsqueeze(2).to_broadcast([128, 4, 64])
            rstd_b = st4[:, 5, :].unsqueeze(2).to_broadcast([128, 4, 64])
            tt('dve', y3, y3, mean_b, OP.subtract, ['ysb', 'st4'], ['ysb'])
            tt('dve', y3, y3, rstd_b, OP.mult, ['ysb', 'st4'], ['ysb'])
            tt('pool', ysb[:], ysb[:], rowb[:, 0:256], OP.mult, ['ysb', 'rowb'], ['ysb'])
            tt('pool', ysb[:], ysb[:], rowb[:, 256:512], OP.add, ['ysb', 'rowb'], ['ysb'])
            rk_b = rksc[:].unsqueeze(2).to_broadcast([128, 4, 64])
            v3 = Vr[:, c, :].rearrange("p (h i) -> p h i", h=4)
            tt('dve', q3, v3, rk_b, OP.mult, ['Vr', 'rksc'], ['ysq'])
            tt('dve', ysb[:], ysb[:], ysq[:], OP.add, ['ysb', 'ysq'], ['ysb'])
            tt('dve', ysb[:], ysb[:], gtok[:, c, :], OP.mult, ['ysb', 'gtok'], ['ysb'])
            bt_ = nb()
            for p_ in range(2):
                tr(ps[bt_][:, p_ * 128:(p_ + 1) * 128], ysb[:, p_ * 128:(p_ + 1) * 128], identF, ['ysb', 'CF'], ['ps%d' % bt_])
            cp('act', mixR[:, :, c * 128:(c + 1) * 128], ps[bt_][:, 0:256].rearrange("p (a t) -> p a t", a=2),
               ['ps%d' % bt_], ['mixR'])
        for p_ in range(2):
            dma(agin[2 + p_][:, t0:t0 + TS], mixR[:, p_, :], ['mixR'], ['agin'], 'mixR')

    P.flush()
    es.close()
    if dbg == 'p1':
        if os.environ.get('NOFINAL') != '1':
            P.final_wait()
        ges.close()
        return nc

    ccs = ges.enter_context(nc.semaphore('ccs'))
    with nc.Block() as blk:
        def body(g):
            for (semh, val, key) in P.fence:
                if val > 0:
                    g.wait_ge(semh, val)
            for f in range(4):
                g.collective_compute("AllGather", OP.bypass, replica_groups=[[0, 1, 2, 3], [4, 5, 6, 7]],
                                     ins=[agin[f]], outs=[agout[f]]).then_inc(ccs)
                g.wait_ge(ccs, f + 1)
        blk.gpsimd(body)
    P.fence = P.fence + [(ccs, 4, 'ccs')]

    es2 = ExitStack()
    acc = sb(es2, 'acc', [128, 8, D])
    x1T = sb(es2, 'x1T', [128, 16, 1024], BF16)
    G = sb(es2, 'G', [128, 8, 32])
    GTfull = sb(es2, 'GT', [128, 1024])
    GT = GTfull[0:32, :]
    esa = ExitStack()
    mixT = sb(esa, 'mixT', [128, 16, 1024], BF16)
    WoB = [sb(esa, 'WoB%d' % i, [128, 16, 256], BF16) for i in range(2)]
    wos = [sb(esa, 'wos%d' % i, [128, 4, 256]) for i in range(2)]
    xr = [sb(esa, 'xr%d' % i, [128, 256]) for i in range(2)]
    lnb = sb(esa, 'lnb', [128, 2, D])
    x1Tf = sb(esa, 'x1Tf', [128, 16, 128])
    rwS = sb(esa, 'rwS', [128, 16, 32])
    rbS = sb(esa, 'rbS', [128, 32])
    stl = sb(esa, 'stl', [128, 16])
    lg = sb(esa, 'lg', [128, 32])
    ex = sb(esa, 'ex', [128, 32])
    mk = sb(esa, 'mk', [128, 32])
    m8b = sb(esa, 'm8b', [128, 8])
    junk = sb(esa, 'junk', [128, D], BF16)

    def issue_mixT(eng):
        pid = eng.partition_id()
        off = (pid % 4) * 1024
        last = None
        for fc in range(16):
            last = eng.dma_start(out=mixT[:, fc, :], in_=agout[fc % 4][(fc // 4) * 128:(fc // 4 + 1) * 128, bass.ds(off, 1024)])
            if fc < 15:
                last.then_inc(P.dsem['mixT'], 16)
        return last
    P.dsem['mixT'] = ges.enter_context(nc.semaphore('d_mixT'))
    P.dcnt['mixT'] = 15 * 16
    P.op('sp', issue_mixT, (), ['mixT'], dma='mixT')
    dma(lnb[:, 0, :], lnv[:, 0:D].partition_broadcast(128), (), ['lnb'], 'lnb')
    dma(lnb[:, 1, :], lnv[:, D:2 * D].partition_broadcast(128), (), ['lnbB'], 'lnb')
    dma(rwS[:], router_w.rearrange("(c p) e -> p c e", p=128), (), ['rwS'], 'rwS')
    dma(rbS[:], router_b.partition_broadcast(128), (), ['rbS'], 'rbS')

    for mt in range(8):
        wb, wbn = WoB[mt % 2], 'WoB%d' % (mt % 2)
        for q4 in range(4):
            st_, sn = wos[q4 % 2], 'wos%d' % (q4 % 2)
            dma(st_[:], w_out[q4 * 512:(q4 + 1) * 512, mt * 256:(mt + 1) * 256].rearrange("(c p) m -> p c m", p=128),
                (), [sn], sn)
            cp('act' if q4 % 2 == 0 else 'pool', wb[:, q4 * 4:(q4 + 1) * 4, :], st_[:], [sn], [wbn])
        for tc in range(8):
            xb_, xn = xr[tc % 2], 'xr%d' % (tc % 2)
            dma(xb_[:], xres[tc * 128:(tc + 1) * 128, mt * 256:(mt + 1) * 256], (), [xn], xn)
            b = nb()
            for fc in range(16):
                mm(ps[b][:, 0:256], mixT[:, fc, tc * 128:(tc + 1) * 128], wb[:, fc, :], fc == 0, fc == 15,
                   ['mixT', wbn], ['ps%d' % b])
            stt('dve', acc[:, tc, mt * 256:(mt + 1) * 256], xb_[:], float(ALPHA), ps[b][:, 0:256], OP.mult, OP.add,
                [xn, 'ps%d' % b], ['acc%d' % tc])

    def layernorm(tc, gi, stl_, junk_, lnb_, lnbn):
        a = acc[:, tc, :]
        an = 'acc%d' % tc
        P.op('dve', lambda e: e.tensor_reduce(out=stl_[:, 0:1], in_=a, axis=AX.X, op=OP.add), [an], ['stl'])
        act(junk_[:], a, AF.Square, [an], ['junk', 'stl2'], accum=stl_[:, 1:2])
        ts('dve', stl_[:, 2:3], stl_[:, 0:1], 1.0 / D, None, OP.mult, None, ['stl'], ['stl'])
        tt('dve', stl_[:, 3:4], stl_[:, 2:3], stl_[:, 2:3], OP.mult, ['stl'], ['stl'])
        stt('dve', stl_[:, 4:5], stl_[:, 1:2], 1.0 / D, stl_[:, 3:4], OP.mult, OP.subtract, ['stl', 'stl2'], ['stl'])
        ts('dve', stl_[:, 5:6], stl_[:, 4:5], 1e-5, None, OP.add, None, ['stl'], ['stl'])
        act(stl_[:, 5:6], stl_[:, 5:6], AF.Ln, ['stl'], ['stl'])
        act(stl_[:, 5:6], stl_[:, 5:6], AF.Exp, ['stl'], ['stl'], scale=-0.5)
        ts('dve', a, a, stl_[:, 2:3], stl_[:, 5:6], OP.subtract, OP.mult, [an, 'stl'], [an])
        tt('pool', a, a, lnb_[:, gi, :], OP.mult, [an, lnbn, lnbn + 'B'], [an])
        tt('dve', a, a, lnb_[:, gi + 1, :], OP.add, [an, lnbn, lnbn + 'B'], [an])

    for tc in range(8):
        mset('dve', stl[:, 1:2], 0.0, ['stl2'])
        layernorm(tc, 0, stl, junk, lnb, 'lnb')
        an = 'acc%d' % tc
        for q4 in range(4):
            b = nb()
            for k4 in range(4):
                dc = q4 * 4 + k4
                tr(ps[b][:, k4 * 128:(k4 + 1) * 128], acc[:, tc, dc * 128:(dc + 1) * 128], identF, [an, 'CF'], ['ps%d' % b])
            cp('act', x1T[:, q4 * 4:(q4 + 1) * 4, tc * 128:(tc + 1) * 128],
               ps[b][:].rearrange("p (c t) -> p c t", c=4), ['ps%d' % b], ['x1T'])
            cp('dve', x1Tf[:, q4 * 4:(q4 + 1) * 4, :], ps[b][:].rearrange("p (c t) -> p c t", c=4), ['ps%d' % b], ['x1Tf'])
        b = nb()
        for dc in range(16):
            mm(ps[b][:, 0:32], x1Tf[:, dc, :], rwS[:, dc, :], dc == 0, dc == 15, ['x1Tf', 'rwS'], ['ps%d' % b])
        tt('dve', lg[:], ps[b][:, 0:32], rbS[:], OP.add, ['ps%d' % b, 'rbS'], ['lg'])
        P.op('dve', lambda e: e.max(out=m8b[:], in_=lg[:]), ['lg'], ['m8b'])
        ts('dve', mk[:], lg[:], m8b[:, 3:4], None, OP.is_ge, None, ['lg', 'm8b'], ['mk'])
        ts('dve', stl[:, 8:9], m8b[:, 0:1], -1.0, None, OP.mult, None, ['m8b'], ['stl3'])
        act(ex[:], lg[:], AF.Exp, ['lg', 'stl3'], ['ex'], bias=stl[:, 8:9])
        tt('dve', ex[:], ex[:], mk[:], OP.mult, ['ex', 'mk'], ['ex'])
        P.op('dve', lambda e: e.tensor_reduce(out=stl[:, 9:10], in_=ex[:], axis=AX.X, op=OP.add), ['ex'], ['stl3'])
        P.op('dve', lambda e: e.reciprocal(out=stl[:, 10:11], in_=stl[:, 9:10]), ['stl3'], ['stl3'])
        ts('dve', G[:, tc, :], ex[:], stl[:, 10:11], None, OP.mult, None, ['ex', 'stl3'], ['G'])
        b = nb()
        tr(ps[b][0:32, 0:128], G[:, tc, :], identF, ['G', 'CF'], ['ps%d' % b])
        cp('act', GT[:, tc * 128:(tc + 1) * 128], ps[b][0:32, 0:128], ['ps%d' % b], ['GT', 'GTa', 'GTb'])
        ts('pool', acc[:, tc, :], acc[:, tc, :], float(ALPHA), None, OP.mult, None, [an], [an])
    if dbg == 'p2a':
        for tc in range(8):
            dma(out[tc * 128:(tc + 1) * 128, :], acc[:, tc, :], ['acc%d' % tc], ['out%d' % tc], 'out')
    P.flush()
    esa.close()
    if dbg == 'p2a':
        P.final_wait()
        es2.close()
        ges.close()
        return nc

    esb = ExitStack()
    AT = sb(esb, 'AT', [128, 16, 1024], BF16)
    NSTG = 2
    wsg = [sb(esb, 'wsg%d' % i, [128, 8, 256]) for i in range(NSTG)]
    WgB = [sb(esb, 'WgB%d' % i, [128, 16, 256], BF16) for i in range(2)]
    WdB = [sb(esb, 'WdB%d' % i, [128, 16, 256], BF16) for i in range(2)]
    bguS = sb(esb, 'bguS', [128, 1024])
    gt_ = [sb(esb, 'gt%d' % i, [128, 512]) for i in range(1)]
    sg_ = [sb(esb, 'sg%d' % i, [128, 512]) for i in range(1)]
    ut_ = [sb(esb, 'ut%d' % i, [128, 512]) for i in range(1)]
    bdS = ut_[0][0:32, :]
    dma(bguS[:], bgu_p, (), ['bguS'], 'bguS')
    bgu3 = bguS[:].rearrange("p (e c) -> p e c", e=32)
    ts('dve', bgu3[:, :, 16:32], bgu3[:, :, 16:32], 1.0, None, OP.add, None, ['bguS'], ['bguS'])
    for mq in range(4):
        dma(bdS, b_down[:, mq * 512:(mq + 1) * 512], (), ['ut0'], 'bdS')
        for tc in range(8):
            b = nb()
            mm(ps[b][:], GT[:, tc * 128:(tc + 1) * 128], bdS, True, True, ['GT', 'GTa', 'GTb', 'ut0'], ['ps%d' % b])
            tt('dve', acc[:, tc, mq * 512:(mq + 1) * 512], acc[:, tc, mq * 512:(mq + 1) * 512], ps[b][:], OP.add,
                   ['ps%d' % b, 'acc%d' % tc], ['acc%d' % tc])
    sgi = 0
    it = 0
    _NOW = os.environ.get('NOWDMA') == '1'
    for e_ in range(32):
        for fc in range(16):
            wg, wgn = WgB[it % 2], 'WgB%d' % (it % 2)
            for q8 in (range(2) if not _NOW else []):
                st_, sn = wsg[sgi % NSTG], 'wsg%d' % (sgi % NSTG)
                for half in range(2):
                    c0 = half * 2048 + fc * 128
                    dma(st_[:, :, half * 128:(half + 1) * 128],
                        w_gu[e_, q8 * 1024:(q8 + 1) * 1024, c0:c0 + 128].rearrange("(c p) m -> p c m", p=128),
                        (), [sn + 'ab'[half]], sn)
                cp('act' if sgi % 2 == 0 else 'pool', wg[:, q8 * 8:(q8 + 1) * 8, :], st_[:], [sn + 'a', sn + 'b'], [wgn])
                sgi += 1
            for th in range(2):
                tsl = slice(th * 512, (th + 1) * 512)
                bg = nb()
                for dc in range(16):
                    mm(ps[bg][:], wg[:, dc, 0:128], x1T[:, dc, tsl], dc == 0, dc == 15, [wgn, 'x1T'], ['ps%d' % bg])
                bu = nb()
                for dc in range(16):
                    mm(ps[bu][:], wg[:, dc, 128:256], x1T[:, dc, tsl], dc == 0, dc == 15, [wgn, 'x1T'], ['ps%d' % bu])
                k2 = (fc * 2 + th) % 2
                if k2 == 0:
                    g_, gn = gt_[0][:], 'gt0'
                    u_, un = ut_[0][:], 'ut0'
                else:
                    g_, gn = GTfull[:, 0:512], 'GTa'
                    u_, un = GTfull[:, 512:1024], 'GTb'
                s__, sn_ = sg_[0], 'sg0'
                bcol_g = bguS[:, e_ * 32 + fc:e_ * 32 + fc + 1]
                bcol_u = bguS[:, e_ * 32 + 16 + fc:e_ * 32 + 16 + fc + 1]
                ts('dve', g_, ps[bg][:], bcol_g, 7.0, OP.add, OP.min, ['ps%d' % bg, 'bguS'], [gn])
                act(s__[:], g_, AF.Sigmoid, [gn], [sn_], scale=1.702)
                ts('dve', u_, ps[bu][:], bcol_u, 8.0, OP.add, OP.min, ['ps%d' % bu, 'bguS'], [un])
                tt('pool', g_, g_, s__[:], OP.mult, [gn, sn_], [gn])
                stt('dve', AT[:, fc, tsl], u_, -6.0, g_, OP.max, OP.mult, [un, gn], ['AT'])
            it += 1
        for mt in range(8):
            wd, wdn = WdB[mt % 2], 'WdB%d' % (mt % 2)
            for q8 in (range(2) if not _NOW else []):
                st_, sn = wsg[sgi % NSTG], 'wsg%d' % (sgi % NSTG)
                dma(st_[:], w_down[e_, q8 * 1024:(q8 + 1) * 1024, mt * 256:(mt + 1) * 256].rearrange("(c p) m -> p c m", p=128),
                    (), [sn + 'a', sn + 'b'], sn)
                cp('act' if sgi % 2 == 0 else 'pool', wd[:, q8 * 8:(q8 + 1) * 8, :], st_[:], [sn + 'a', sn + 'b'], [wdn])
                sgi += 1
            for tc in range(8):
                b = nb()
                for fc in range(16):
                    mm(ps[b][:, 0:256], AT[:, fc, tc * 128:(tc + 1) * 128], wd[:, fc, :], fc == 0, fc == 15,
                       ['AT', wdn], ['ps%d' % b])
                msl = slice(mt * 256, (mt + 1) * 256)
                stt('dve', acc[:, tc, msl], ps[b][:, 0:256], G[:, tc, e_:e_ + 1], acc[:, tc, msl], OP.mult, OP.add,
                    ['ps%d' % b, 'G', 'acc%d' % tc], ['acc%d' % tc])
    P.flush()
    esb.close()

    esc = ExitStack()
    lnb2 = sb(esc, 'lnb2', [128, 2, D])
    stl2 = sb(esc, 'stl2', [128, 16])
    junk2 = sb(esc, 'junk2', [128, D], BF16)
    dma(lnb2[:, 0, :], lnv[:, 2 * D:3 * D].partition_broadcast(128), (), ['lnb2'], 'lnb2')
    dma(lnb2[:, 1, :], lnv[:, 3 * D:4 * D].partition_broadcast(128), (), ['lnb2B'], 'lnb2')
    for tc in range(8):
        mset('dve', stl2[:, 1:2], 0.0, ['stl2'])
        layernorm(tc, 0, stl2, junk2, lnb2, 'lnb2')
        dma(out[tc * 128:(tc + 1) * 128, :], acc[:, tc, :], ['acc%d' % tc], ['out%d' % tc], 'out')
    P.flush()
    P.final_wait()
    esc.close()
    es2.close()
    ges.close()
    return nc


def _consts():
    inv = (10000.0 ** (-np.arange(0, 128, 2, dtype=np.float32) / 128.0)).astype(np.float32)
    ang = np.arange(T, dtype=np.float32)[:, None] * inv[None, :]
    cos = np.concatenate([np.cos(ang), np.cos(ang)], -1).T.astype(np.float32)
    sin = np.concatenate([np.sin(ang), np.sin(ang)], -1).T.astype(np.float32)
    cf = np.zeros((128, 128 + 3 * 512 + 128 + 3 * 256), np.float32)
    cf[:, 0:128] = np.eye(128)
    i = np.arange(128)
    su = (i[:, None] < i[None, :]).astype(np.float32)
    sl = (i[:, None] > i[None, :]).astype(np.float32)
    ui = (i[:, None] <= i[None, :]).astype(np.float32)
    cf[:, 128:640] = np.tile(su, (1, 4))
    cf[:, 640:1152] = np.tile(sl, (1, 4))
    cf[:, 1152:1664] = np.tile(ui, (1, 4))
    bo = np.zeros((128, 128), np.float32)
    bo[0:64, 0:64] = 1
    bo[64:, 64:] = 1
    cf[:, 1664:1792] = bo
    n = np.arange(16)
    for cur in range(16):
        cf[:, 1792 + cur * 16:1792 + (cur + 1) * 16] = np.where(n < cur, 0.0, NEG)[None, :]
        cf[:, 2048 + cur * 16:2048 + (cur + 1) * 16] = np.where(n == cur, 0.0, NEG)[None, :]
        cf[:, 2304 + cur * 16:2304 + (cur + 1) * 16] = np.where(n > cur, NEG, 0.0)[None, :]
    cb = np.zeros((128, 128 + 128 + 128 + 512 + 2048), np.float32)
    cb[:, 0:128] = np.eye(128)
    rot = np.zeros((128, 128), np.float32)
    for dp in range(64):
        rot[dp + 64, dp] = -1.0
    for dp in range(64, 128):
        rot[dp - 64, dp] = 1.0
    cb[:, 128:256] = rot
    cb[:, 256:384] = 1.0
    q = np.arange(256)
    for kt in range(2):
        kk = kt * 128 + i
        cb[:, 384 + kt * 256:384 + (kt + 1) * 256] = np.where(kk[:, None] <= q[None, :], 0.0, NEG)
    for nn in range(16):
        cb[nn, 896 + nn * 128:896 + (nn + 1) * 128] = 1.0
    return cos, sin, cf, cb.astype(ml_dtypes.bfloat16)


_NC = None


def _prep(inputs, p1only=False, p2a=False):
    f = lambda a: np.ascontiguousarray(np.asarray(a, dtype=np.float32))
    x = f(inputs['x'])
    w_in = f(inputs['w_in'])[0]
    mu = f(inputs['mu_shift'])[0]
    cos, sin, cf, cb = _consts()
    if p1only:
        shared = dict(cosT=cos, sinT=sin, cf32=cf, cbf=cb)
    else:
        w_out = f(inputs['w_out'])[0]
        perm = np.concatenate([np.concatenate([np.arange(256 * g, 256 * g + 256), 1024 + np.arange(256 * g, 256 * g + 256)])
                               for g in range(4)])
        w_out_p = np.ascontiguousarray(w_out[perm])
        lnv = np.concatenate([f(inputs['ln1_g'])[0], f(inputs['ln1_b'])[0], f(inputs['ln2_g'])[0], f(inputs['ln2_b'])[0]])[None, :]
        if p2a:
            shared = dict(w_out_p=w_out_p, lnv=np.ascontiguousarray(lnv), router_w=f(inputs['router_w'])[0],
                          router_b=f(inputs['router_b']), cosT=cos, sinT=sin, cf32=cf, cbf=cb)
        else:
            b_gu = f(inputs['b_gu'])[0]
            bgu_p = np.ascontiguousarray(b_gu.reshape(32, 32, 128).transpose(2, 0, 1).reshape(128, 1024))
            shared = dict(w_out_p=w_out_p, lnv=np.ascontiguousarray(lnv), router_w=f(inputs['router_w'])[0],
                          router_b=f(inputs['router_b']), w_gu=f(inputs['w_gu'])[0], bgu_p=bgu_p,
                          w_down=f(inputs['w_down'])[0], b_down=f(inputs['b_down'])[0],
                          cosT=cos, sinT=sin, cf32=cf, cbf=cb)
    maps = []
    for c in range(8):
        b, g = c // 4, c % 4
        sl = slice(256 * g, 256 * g + 256)
        cols = np.concatenate([np.arange(256 * g, 256 * g + 256), 1024 + np.arange(256 * g, 256 * g + 256),
                               2048 + np.arange(256 * g, 256 * g + 256), 3072 + np.arange(256 * g, 256 * g + 256),
                               4096 + np.arange(256 * g, 256 * g + 256), 5120 + np.arange(256 * g, 256 * g + 256),
                               np.arange(6144, 6432)])
        pvec = np.zeros((128, 20), np.float32)

        def two(v):
            return v[sl].reshape(2, 128).T
        pvec[:, 0:2] = two(mu[0:1024])
        pvec[:, 2:4] = two(mu[1024:2048])
        pvec[:, 4] = mu[3072:3200]
        pvec[:, 5] = mu[3200:3328]
        pvec[0:32, 6] = mu[3328:3360]
        pvec[:, 7:9] = two(f(inputs['w0'])[0])
        pvec[:, 9:11] = two(f(inputs['a0'])[0])
        pvec[:, 11:13] = two(f(inputs['k_k'])[0])
        pvec[:, 13:15] = two(f(inputs['k_a'])[0])
        pvec[:, 15:17] = two(f(inputs['r_k'])[0].reshape(-1))
        rowv = np.concatenate([f(inputs['lnx_g'])[0][sl], f(inputs['lnx_b'])[0][sl]])[None, :]
        m = dict(shared)
        if not p1only:
            m['xres'] = np.ascontiguousarray(x[b, 1024 * g:1024 * (g + 1)])
        m.update(x=x[b],
                 w_in_c=np.ascontiguousarray(w_in[:, cols]),
                 mu_rv=np.ascontiguousarray(mu[2048:3072][sl][None, :]), pvec=pvec, rowv=np.ascontiguousarray(rowv),
                 w_up_c=np.ascontiguousarray(f(inputs['w_up'])[0][:, sl]),
                 a_up_c=np.ascontiguousarray(f(inputs['a_up'])[0][:, sl]),
                 g_up_c=np.ascontiguousarray(f(inputs['g_up'])[0][:, sl]))
        maps.append(m)
    return maps


def kernel(**inputs):
    global _NC
    if _NC is None:
        _NC = build()
    maps = _prep(inputs)
    res = run_bass_kernel_spmd(_NC, maps, core_ids=list(range(8)))
    outs = [np.asarray(r['out'], dtype=np.float32) for r in res.results]
    return np.stack(outs, 0).reshape(2, 4096, 2048)
```
